# Optimizing a Trainium2 kernel written in Bass

```python
import jax, jax.numpy as jnp
from jax import lax
import numpy as np

D_MODEL = 1024
BATCH = 16
SEQ = 2048
DEPTH = 2

MIX_WIDTH = D_MODEL
MLSTM_WIDTH = D_MODEL // 2
MLSTM_HEADS = 4
MLSTM_HEAD_DIM = MLSTM_WIDTH // MLSTM_HEADS
CHUNK = 64
CONV_WIDTH = 4
POOL_WIDTH = MIX_WIDTH - MLSTM_WIDTH
POOL_WINDOWS = (2, 4, 8, 16)
POOL_GROUP = POOL_WIDTH // len(POOL_WINDOWS)
N_IN = 4 * MLSTM_WIDTH + 2 * MLSTM_HEADS + POOL_WIDTH
MEM_LEN = 256
XATTN_HEADS = 4
XATTN_HEAD_DIM = D_MODEL // XATTN_HEADS
N_EXPERTS = 16
N_GROUPS = 4
EXPERTS_PER_GROUP = N_EXPERTS // N_GROUPS
TOP_K = 2
D_EXPERT = D_MODEL // 4
MOE_BLOCK = 128
DEEPNORM_ALPHA = (2 * DEPTH) ** 0.25
DEEPNORM_BETA = (8 * DEPTH) ** -0.25
LN_EPS = 1e-5

kernel_name = "hybrid_mlstm_pool_memxattn_groupmoe_deepnorm"


def layer_norm(x, g, b):
    xf = x.astype(jnp.float32)
    mu = xf.mean(-1, keepdims=True)
    var = jnp.square(xf - mu).mean(-1, keepdims=True)
    return ((xf - mu) * lax.rsqrt(var + LN_EPS) * g + b).astype(x.dtype)


def causal_depthwise_conv(u, w):
    S = u.shape[1]
    K = w.shape[0]
    up = jnp.pad(u, ((0, 0), (K - 1, 0), (0, 0)))
    return sum(up[:, j:j + S] * w[j] for j in range(K))


def mlstm_chunkwise(q, k, v, i_pre, f_pre):
    B, S, H, dh = q.shape
    L = CHUNK
    NC = S // L

    def chunk(t):
        t = t.reshape((B, NC, L, H) + t.shape[3:])
        return jnp.moveaxis(t, 3, 1)

    q = chunk(q.astype(jnp.float32)) * dh ** -0.5
    k = chunk(k.astype(jnp.float32))
    v = chunk(v.astype(jnp.float32))
    ig = chunk(i_pre.astype(jnp.float32))
    b = jnp.cumsum(jax.nn.log_sigmoid(chunk(f_pre.astype(jnp.float32))), axis=-1)
    b_last = b[..., -1]

    w_state = b_last[..., None] - b + ig
    m_loc = w_state.max(-1)
    a = jnp.exp(w_state - m_loc[..., None])
    C_loc = jnp.einsum('bhcl,bhcld,bhcle->bhcde', a, k, v)
    n_loc = jnp.einsum('bhcl,bhcld->bhcd', a, k)

    def step(carry, inp):
        C, n, m = carry
        Cl, nl, ml, bl = inp
        m_new = jnp.maximum(bl + m, ml)
        s_old = jnp.exp(bl + m - m_new)
        s_new = jnp.exp(ml - m_new)
        C_new = s_old[..., None, None] * C + s_new[..., None, None] * Cl
        n_new = s_old[..., None] * n + s_new[..., None] * nl
        return (C_new, n_new, m_new), (C, n, m)

    init = (jnp.zeros((B, H, dh, dh), jnp.float32),
            jnp.zeros((B, H, dh), jnp.float32),
            jnp.zeros((B, H), jnp.float32))
    xs = tuple(jnp.moveaxis(t, 2, 0) for t in (C_loc, n_loc, m_loc, b_last))
    _, (C_prev, n_prev, m_prev) = lax.scan(step, init, xs)
    C_prev = jnp.moveaxis(C_prev, 0, 2)
    n_prev = jnp.moveaxis(n_prev, 0, 2)
    m_prev = jnp.moveaxis(m_prev, 0, 2)

    causal = jnp.tril(jnp.ones((L, L), dtype=bool))
    log_d = jnp.where(causal, b[..., :, None] - b[..., None, :] + ig[..., None, :], -jnp.inf)
    log_inter = b + m_prev[..., None]
    m_t = jnp.maximum(log_d.max(-1), log_inter)
    p = jnp.exp(log_d - m_t[..., None]) * jnp.einsum('bhcld,bhcsd->bhcls', q, k)
    inter = jnp.exp(log_inter - m_t)
    num = (jnp.einsum('bhcls,bhcse->bhcle', p, v)
           + inter[..., None] * jnp.einsum('bhcld,bhcde->bhcle', q, C_prev))
    den = p.sum(-1) + inter * jnp.einsum('bhcld,bhcd->bhcl', q, n_prev)
    h = num / jnp.maximum(jnp.abs(den), jnp.exp(-m_t))[..., None]
    return jnp.moveaxis(h, 1, 3).reshape(B, S, H, dh)


def multiscale_pool(u):
    S = u.shape[1]
    pos = jnp.arange(1, S + 1, dtype=jnp.float32)[:, None]
    outs = []
    for g, w in enumerate(POOL_WINDOWS):
        ug = u[..., g * POOL_GROUP:(g + 1) * POOL_GROUP].astype(jnp.float32)
        cs = jnp.cumsum(ug, axis=1)
        lagged = jnp.pad(cs, ((0, 0), (w, 0), (0, 0)))[:, :S]
        outs.append((cs - lagged) / jnp.minimum(pos, w) - ug)
    return jnp.stack(outs, axis=2)


def hybrid_mixer(x, w_in, b_i, b_f, conv_qk, head_norm_g, pool_w, pool_scale, w_out):
    B, S, _ = x.shape
    M, H, dh = MLSTM_WIDTH, MLSTM_HEADS, MLSTM_HEAD_DIM
    z = x @ w_in
    qk = jax.nn.silu(causal_depthwise_conv(z[..., :2 * M], conv_qk))
    q = qk[..., :M].reshape(B, S, H, dh)
    k = qk[..., M:].reshape(B, S, H, dh)
    v = z[..., 2 * M:3 * M].reshape(B, S, H, dh)
    o_pre = z[..., 3 * M:4 * M]
    i_pre = z[..., 4 * M:4 * M + H] + b_i
    f_pre = z[..., 4 * M + H:4 * M + 2 * H] + b_f
    u = z[..., 4 * M + 2 * H:]

    h = mlstm_chunkwise(q, k, v, i_pre, f_pre)
    mu = h.mean(-1, keepdims=True)
    var = jnp.square(h - mu).mean(-1, keepdims=True)
    h = (h - mu) * lax.rsqrt(var + LN_EPS) * head_norm_g.reshape(H, dh).astype(jnp.float32)
    h = h.reshape(B, S, M) * jax.nn.sigmoid(o_pre.astype(jnp.float32))

    pooled = multiscale_pool(u)
    pm = jnp.einsum('bsgc,gcd->bsgd', pooled, pool_w.astype(jnp.float32))
    pm = pm.reshape(B, S, POOL_WIDTH) * pool_scale.astype(jnp.float32)

    mixed = jnp.concatenate([h, pm], axis=-1).astype(x.dtype)
    return mixed @ w_out


def memory_cross_attention(x, mem, w_q, w_kv, w_o):
    B, S, D = x.shape
    Mlen = mem.shape[1]
    q = (x @ w_q).reshape(B, S, XATTN_HEADS, XATTN_HEAD_DIM)
    kv = (mem @ w_kv).reshape(B, Mlen, 2, XATTN_HEADS, XATTN_HEAD_DIM)
    s = jnp.einsum('bshd,bmhd->bhsm', q, kv[:, :, 0]).astype(jnp.float32) * XATTN_HEAD_DIM ** -0.5
    p = jax.nn.softmax(s, axis=-1).astype(x.dtype)
    o = jnp.einsum('bhsm,bmhd->bshd', p, kv[:, :, 1]).reshape(B, S, D)
    return o @ w_o


def grouped_moe(x, router_w, router_bias, w_gate, w_up, w_down):
    B, S, D = x.shape
    T = B * S
    A = T * TOP_K
    xt = x.reshape(T, D)
    scores = jax.nn.sigmoid((xt @ router_w).astype(jnp.float32))
    sel = scores + router_bias.astype(jnp.float32)
    grp_score = lax.top_k(sel.reshape(T, N_GROUPS, EXPERTS_PER_GROUP), 2)[0].sum(-1)
    best = jnp.argmax(grp_score, axis=-1)
    in_grp = (jnp.arange(N_EXPERTS) // EXPERTS_PER_GROUP)[None, :] == best[:, None]
    _, idx = lax.top_k(jnp.where(in_grp, sel, -jnp.inf), TOP_K)
    gate = jnp.take_along_axis(scores, idx, axis=-1)
    gate = gate / gate.sum(-1, keepdims=True)

    e = idx.reshape(A).astype(jnp.int32)
    e_sorted, order = lax.sort((e, jnp.arange(A, dtype=jnp.int32)), num_keys=1)
    counts = jnp.bincount(e, length=N_EXPERTS)
    padded = (counts + MOE_BLOCK - 1) // MOE_BLOCK * MOE_BLOCK
    pend = jnp.cumsum(padded)
    pstart = pend - padded
    start = jnp.cumsum(counts) - counts
    dest_sorted = pstart[e_sorted] + jnp.arange(A, dtype=jnp.int32) - start[e_sorted]
    R = A + N_EXPERTS * MOE_BLOCK
    NB = R // MOE_BLOCK
    buf = jnp.zeros((R, D), x.dtype).at[dest_sorted].set(xt[order // TOP_K])
    blk_e = jnp.minimum(jnp.searchsorted(pend, jnp.arange(NB, dtype=jnp.int32) * MOE_BLOCK,
                                         side='right'), N_EXPERTS - 1)

    def expert_block(args):
        xb, ei = args
        hb = jax.nn.silu(xb @ w_gate[ei]) * (xb @ w_up[ei])
        return hb @ w_down[ei]

    yb = lax.map(expert_block, (buf.reshape(NB, MOE_BLOCK, D), blk_e)).reshape(R, D)
    dest = jnp.zeros((A,), jnp.int32).at[order].set(dest_sorted)
    y = (yb[dest].reshape(T, TOP_K, D) * gate[..., None].astype(x.dtype)).sum(1)
    return y.reshape(B, S, D)


def setup_inputs(seed: int = 0) -> dict:
    key = jax.random.key(seed)
    ks = jax.random.split(key, 24)
    f32 = jnp.float32
    D, L_ = D_MODEL, DEPTH
    beta = DEEPNORM_BETA
    nrm = lambda k, shape, s: jax.random.normal(k, shape, f32) * s
    col_scale = jnp.asarray(np.concatenate([
        np.ones(2 * MLSTM_WIDTH), np.full(MLSTM_WIDTH, beta), np.ones(MLSTM_WIDTH + 2 * MLSTM_HEADS),
        np.full(POOL_WIDTH, beta)]).astype(np.float32))
    kv_scale = jnp.asarray(np.concatenate([np.ones(D), np.full(D, beta)]).astype(np.float32))
    return {
        "x": nrm(ks[0], (BATCH, SEQ, D), 1.0),
        "mem": nrm(ks[1], (BATCH, MEM_LEN, D), 1.0),
        "w_in": nrm(ks[2], (L_, D, N_IN), D ** -0.5) * col_scale,
        "b_i": nrm(ks[3], (L_, MLSTM_HEADS), 0.1),
        "b_f": jnp.linspace(3.0, 6.0, MLSTM_HEADS, dtype=f32)[None, :] + nrm(ks[4], (L_, MLSTM_HEADS), 0.1),
        "conv_qk": nrm(ks[5], (L_, CONV_WIDTH, 2 * MLSTM_WIDTH), CONV_WIDTH ** -0.5),
        "head_norm_g": 1.0 + nrm(ks[6], (L_, MLSTM_WIDTH), 0.05),
        "pool_w": nrm(ks[7], (L_, len(POOL_WINDOWS), POOL_GROUP, POOL_GROUP), POOL_GROUP ** -0.5),
        "pool_scale": 1.0 + nrm(ks[8], (L_, POOL_WIDTH), 0.1),
        "w_mix_out": nrm(ks[9], (L_, MIX_WIDTH, D), MIX_WIDTH ** -0.5 * beta),
        "ln_mix_g": 1.0 + nrm(ks[10], (L_, D), 0.05),
        "ln_mix_b": nrm(ks[11], (L_, D), 0.02),
        "w_xq": nrm(ks[12], (L_, D, D), D ** -0.5),
        "w_xkv": nrm(ks[13], (L_, D, 2 * D), D ** -0.5) * kv_scale,
        "w_xo": nrm(ks[14], (L_, D, D), D ** -0.5 * beta),
        "ln_x_g": 1.0 + nrm(ks[15], (L_, D), 0.05),
        "ln_x_b": nrm(ks[16], (L_, D), 0.02),
        "router_w": nrm(ks[17], (D, N_EXPERTS), D ** -0.5),
        "router_bias": nrm(ks[18], (N_EXPERTS,), 0.01),
        "w_gate": nrm(ks[19], (L_, N_EXPERTS, D, D_EXPERT), D ** -0.5 * beta),
        "w_up": nrm(ks[20], (L_, N_EXPERTS, D, D_EXPERT), D ** -0.5 * beta),
        "w_down": nrm(ks[21], (L_, N_EXPERTS, D_EXPERT, D), D_EXPERT ** -0.5 * beta),
        "ln_moe_g": 1.0 + nrm(ks[22], (L_, D), 0.05),
        "ln_moe_b": nrm(ks[23], (L_, D), 0.02),
    }


def reference(x, mem, w_in, b_i, b_f, conv_qk, head_norm_g, pool_w, pool_scale, w_mix_out,
              ln_mix_g, ln_mix_b, w_xq, w_xkv, w_xo, ln_x_g, ln_x_b, router_w, router_bias,
              w_gate, w_up, w_down, ln_moe_g, ln_moe_b):
    for l in range(DEPTH):
        mix = hybrid_mixer(x, w_in[l], b_i[l], b_f[l], conv_qk[l], head_norm_g[l],
                           pool_w[l], pool_scale[l], w_mix_out[l])
        x = layer_norm(DEEPNORM_ALPHA * x + mix, ln_mix_g[l], ln_mix_b[l])
        xa = memory_cross_attention(x, mem, w_xq[l], w_xkv[l], w_xo[l])
        x = layer_norm(DEEPNORM_ALPHA * x + xa, ln_x_g[l], ln_x_b[l])
        ff = grouped_moe(x, router_w, router_bias, w_gate[l], w_up[l], w_down[l])
        x = layer_norm(DEEPNORM_ALPHA * x + ff, ln_moe_g[l], ln_moe_b[l])
    return x
```

```python
import numpy as np
import concourse.bass as bass
import concourse.mybir as mybir
from concourse.bass_utils import run_bass_kernel_spmd

F32 = mybir.dt.float32
BF16 = mybir.dt.bfloat16
AF = mybir.ActivationFunctionType
ALU = mybir.AluOpType
AX = mybir.AxisListType

ENGS = ("pe", "act", "dve", "pool", "sp")
DMA_ENGS = ("sp", "act", "pool")


class Op:
    __slots__ = ("eng", "fn", "idx", "deps", "is_dma", "chan", "chan_cnt", "signal", "sigcnt", "waits")

    def __init__(self, eng, fn, idx):
        self.eng = eng
        self.fn = fn
        self.idx = idx
        self.deps = []
        self.is_dma = False
        self.chan = None
        self.chan_cnt = 0
        self.signal = False
        self.sigcnt = 0
        self.waits = []


class Sched:
    SAME_ENG_WINDOW = 4

    def __init__(self, nc):
        self.nc = nc
        self.ops = {e: [] for e in ENGS}
        self.last_writer = {}
        self.readers = {}
        self.chan_cnt = {}
        self.chan_last = {}

    def _add(self, eng, fn, reads, writes):
        op = Op(eng, fn, len(self.ops[eng]))
        deps = []
        for k in reads:
            w = self.last_writer.get(k)
            if w is not None:
                deps.append(w)
            if k.startswith("ps"):
                deps.extend(r for r in self.readers.get(k, ()) if r.eng != eng)
        for k in writes:
            w = self.last_writer.get(k)
            if w is not None:
                deps.append(w)
            deps.extend(self.readers.get(k, ()))
        op.deps = deps
        for k in writes:
            self.last_writer[k] = op
            self.readers[k] = []
        for k in reads:
            self.readers.setdefault(k, []).append(op)
        self.ops[eng].append(op)
        return op

    def op(self, eng, fn, reads=(), writes=()):
        return self._add(eng, fn, tuple(reads), tuple(writes))

    def dma(self, eng, chan, out, in_, reads=(), writes=()):
        assert eng in DMA_ENGS
        op = self._add(eng, lambda e: e.dma_start(out=out, in_=in_), tuple(reads), tuple(writes))
        op.is_dma = True
        op.chan = chan
        prev = self.chan_last.get(chan)
        if prev is not None:
            op.deps.append(prev)
        self.chan_cnt[chan] = self.chan_cnt.get(chan, 0) + 16
        op.chan_cnt = self.chan_cnt[chan]
        self.chan_last[chan] = op
        return op

    def emit(self):
        nc = self.nc
        for eng in ENGS:
            seen = {e: -1 for e in ENGS}
            seen_ch = {}
            for op in self.ops[eng]:
                need_eng = {}
                need_ch = {}
                for d in op.deps:
                    if d is op:
                        continue
                    if d.is_dma:
                        if seen_ch.get(d.chan, 0) < d.chan_cnt:
                            need_ch[d.chan] = max(need_ch.get(d.chan, 0), d.chan_cnt)
                    else:
                        if d.eng == eng:
                            if eng == "pe":
                                continue
                            if op.idx - d.idx > self.SAME_ENG_WINDOW:
                                continue
                        if seen[d.eng] < d.idx:
                            cur = need_eng.get(d.eng)
                            if cur is None or cur.idx < d.idx:
                                need_eng[d.eng] = d
                waits = []
                for e, d in need_eng.items():
                    d.signal = True
                    seen[e] = d.idx
                    waits.append(d)
                for c, v in need_ch.items():
                    seen_ch[c] = v
                    waits.append((c, v))
                op.waits = waits
        for eng in ENGS:
            c = 0
            for op in self.ops[eng]:
                if op.signal and not op.is_dma:
                    c += 1
                op.sigcnt = c
        sems = {e: nc.alloc_semaphore("sem_" + e) for e in ENGS}
        chsems = {c: nc.alloc_semaphore("ch_" + str(c)) for c in self.chan_cnt}
        self.chsems = chsems
        ops = self.ops

        def run(eng, e):
            for op in ops[eng]:
                for w in op.waits:
                    if isinstance(w, tuple):
                        e.wait_ge(chsems[w[0]], w[1])
                    else:
                        e.wait_ge(sems[w.eng], w.sigcnt)
                inst = op.fn(e)
                if op.is_dma:
                    inst.then_inc(chsems[op.chan], 16)
                elif op.signal:
                    inst.then_inc(sems[eng], 1)

        with nc.Block() as block:
            @block.tensor
            def _(e):
                run("pe", e)

            @block.scalar
            def _(e):
                run("act", e)

            @block.vector
            def _(e):
                run("dve", e)

            @block.gpsimd
            def _(e):
                run("pool", e)

            @block.sync
            def _(e):
                run("sp", e)
        return {e: len(ops[e]) for e in ENGS}


D = 1024
NB = 2
S = 2048
TOK = NB * S
NT = TOK // 128
TPB = S // 128
MEM = 256
M = 512
H = 4
N_IN = 2568
E = 16
DE = 256
ALPHA = float(4 ** 0.25)
EPS = 1e-5
QSCALE = float(128 ** -0.5)
XSCALE = float(256 ** -0.5)

PARAM_SPECS = [
    ("w_in", [2, 1024, 2568]), ("b_i", [2, 4]), ("b_f", [2, 4]), ("conv_qk", [2, 4, 1024]),
    ("head_norm_g", [2, 512]), ("pool_w", [2, 4, 128, 128]), ("pool_scale", [2, 512]),
    ("w_mix_out", [2, 1024, 1024]), ("ln_mix_g", [2, 1024]), ("ln_mix_b", [2, 1024]),
    ("w_xq", [2, 1024, 1024]), ("w_xkv", [2, 1024, 2048]), ("w_xo", [2, 1024, 1024]),
    ("ln_x_g", [2, 1024]), ("ln_x_b", [2, 1024]), ("router_w", [1024, 16]), ("router_bias", [1, 16]),
    ("w_gate", [2, 16, 1024, 256]), ("w_up", [2, 16, 1024, 256]), ("w_down", [2, 16, 256, 1024]),
    ("ln_moe_g", [2, 1024]), ("ln_moe_b", [2, 1024]),
]


class Prog:
    def __init__(self, nlayers=2, stop_after=None, debug=False):
        self.nc = nc = bass.Bass("TRN2", target_bir_lowering=False)
        self.s = Sched(nc)
        self.nlayers = nlayers
        self.stop_after = stop_after
        self.debug = debug
        self.uid = 0
        self.inp = {}
        self.inp["x"] = nc.dram_tensor("x", [TOK, D], F32, kind="ExternalInput").ap()
        self.inp["mem"] = nc.dram_tensor("mem", [NB * MEM, D], F32, kind="ExternalInput").ap()
        for name, shp in PARAM_SPECS:
            self.inp[name] = nc.dram_tensor(name, shp, F32, kind="ExternalInput").ap()
        self.out = nc.dram_tensor("out", [TOK, D], F32, kind="ExternalOutput").ap()
        self.scr = {}
        self.rr = 0
        self.pool_rr = {}
        self.chan_rr = {}

    def name(self, base):
        self.uid += 1
        return "%s_%d" % (base, self.uid)

    def scratch(self, key):
        if key not in self.scr:
            kind = "ExternalOutput" if self.debug else "Internal"
            self.scr[key] = self.nc.dram_tensor("scr_" + key, [TOK, D], F32, kind=kind).ap()
        return self.scr[key]

    def sb(self, stack, base, shape, dtype):
        return stack.enter_context(self.nc.sbuf_tensor(self.name(base), shape, dtype))

    POOLS = None

    def bank(self):
        pools = self.POOLS
        if not pools:
            i = self.rr
            self.rr = (self.rr + 1) % 8
            return i
        name = getattr(self, "cur_pool", None) or "tile"
        lst = pools[name]
        k = self.pool_rr.get(name, 0)
        self.pool_rr[name] = (k + 1) % len(lst)
        return lst[k]

    def chan(self, base, n):
        i = self.chan_rr.get(base, 0)
        self.chan_rr[base] = (i + 1) % n
        return "%s%d" % (base, i)

    def barrier(self):
        s = self.s
        lasts = [s.ops[e][-1] for e in ENGS if s.ops[e]] + list(s.chan_last.values())
        for e in ENGS:
            op = s.op(e, lambda en: en.nop())
            op.deps = [d for d in lasts]

    def setup(self):
        nc, s = self.nc, self.s
        A = nc.alloc_sbuf_tensor
        self.ps = [nc.alloc_psum_tensor("psb%d" % i, [128, 512], F32) for i in range(8)]
        self.psb = [p.bitcast(BF16) for p in self.ps]
        self.xT = A("xT", [128, NB, 8, S], BF16)
        self.ident_f = A("ident_f", [128, 128], F32)
        self.ident_b = A("ident_b", [128, 128], BF16)
        self.triu = A("triu", [128, 128], F32)
        self.maskc = A("maskc", [128, 128], F32)
        self.ones_f = A("ones_f", [128, 128], F32)
        self.wgt = A("wgt", [128, NT, 16], F32)
        self.rw = A("rw", [128, 8, 16], F32)
        self.rb = A("rb", [128, 16], F32)
        self.rdiv = A("rdiv", [128, 4, 16], F32)
        self.iota_i = A("iota_i", [128, 16], mybir.dt.int32)
        self.lng = A("lng", [128, D], F32)
        self.lnb = A("lnb", [128, D], F32)
        self.epsc = A("epsc", [128, 1], F32)
        s.op("pool", lambda e: e.memset(self.epsc[:], EPS), writes=["epsc"])
        s.op("pool", lambda e: e.memset(self.ones_f[:], 1.0), writes=["ones_f"])
        s.op("pool", lambda e: e.affine_select(out=self.ident_f[:], in_=self.ones_f[:], pattern=[[-1, 128]],
                                               compare_op=ALU.is_equal, fill=0.0, base=0, channel_multiplier=1),
             reads=["ones_f"], writes=["ident_f"])
        s.op("pool", lambda e: e.tensor_copy(out=self.ident_b[:], in_=self.ident_f[:]), reads=["ident_f"], writes=["ident_b"])
        s.op("pool", lambda e: e.affine_select(out=self.triu[:], in_=self.ones_f[:], pattern=[[1, 128]],
                                               compare_op=ALU.is_ge, fill=0.0, base=0, channel_multiplier=-1),
             reads=["ones_f"], writes=["triu"])
        s.op("pool", lambda e: e.tensor_scalar(out=self.maskc[:], in0=self.triu[:], scalar1=QSCALE, scalar2=None, op0=ALU.mult),
             reads=["triu"], writes=["maskc"])
        s.op("pool", lambda e: e.iota(self.iota_i[:], pattern=[[1, 16]], base=1, channel_multiplier=0), writes=["iota_i"])
        for g in range(4):
            w = float(2 ** (g + 1))
            s.op("dve", lambda e, g=g, w=w: e.tensor_scalar(out=self.rdiv[:, g, :], in0=self.iota_i[:], scalar1=w, scalar2=None, op0=ALU.min),
                 reads=["iota_i"], writes=["rdiv%d" % g])
            s.op("dve", lambda e, g=g: e.reciprocal(out=self.rdiv[:, g, :], in_=self.rdiv[:, g, :]), reads=["rdiv%d" % g], writes=["rdiv%d" % g])
        s.dma("sp", "c0", self.rw[:], self.inp["router_w"].rearrange("(kc p) n -> p kc n", p=128), writes=["rw"])
        s.dma("sp", "c1", self.rb[:], self.inp["router_bias"][0:1, :].broadcast_to([128, 16]), writes=["rb"])

    def load_x(self):
        s, nc = self.s, self.nc
        import contextlib
        with contextlib.ExitStack() as st:
            xin = [self.sb(st, "xin", [128, D], F32) for _ in range(3)]
            for t in range(NT):
                b, ti = divmod(t, TPB)
                buf = xin[t % 3]
                k = "xin%d" % (t % 3)
                s.dma("sp", self.chan("xl", 3), buf[:], self.inp["x"][t * 128:(t + 1) * 128, :], writes=[k])
                self.transpose_to_xT(buf, [k], b, ti)
            self.barrier()

    def transpose_to_xT(self, src, srckeys, b, ti, xTf=None, xTfkey=None):
        s = self.s
        for hf in range(2):
            bk = self.bank()
            pk = "ps%d" % bk
            pt = self.ps[bk]
            for c in range(4):
                kc = hf * 4 + c
                s.op("pe", lambda e, pt=pt, c=c, kc=kc: e.transpose(pt[:, c * 128:(c + 1) * 128], src[:, kc * 128:(kc + 1) * 128], self.ident_f[:]),
                     reads=list(srckeys) + ["ident_f"], writes=[pk])
            dst = self.xT[:, b, hf * 4:(hf + 1) * 4, ti * 128:(ti + 1) * 128]
            srcv = pt[:].rearrange("p (c t) -> p c t", c=4)
            xk = "xT_%d_%d" % (b, ti)
            if xTf is not None:
                s.op("dve", lambda e, srcv=srcv, hf=hf: e.tensor_copy(out=xTf[:, hf * 4:(hf + 1) * 4, :], in_=srcv), reads=[pk], writes=[xTfkey + str(hf)])
                s.op("act", lambda e, dst=dst, srcv=srcv: e.copy(dst, srcv), reads=[pk], writes=[xk + "_%d" % hf])
            elif hf == 0:
                s.op("dve", lambda e, dst=dst, srcv=srcv: e.tensor_copy(out=dst, in_=srcv), reads=[pk], writes=[xk + "_%d" % hf])
            else:
                s.op("act", lambda e, dst=dst, srcv=srcv: e.copy(dst, srcv), reads=[pk], writes=[xk + "_%d" % hf])

    def xT_keys(self, b, tiles):
        return ["xT_%d_%d_%d" % (b, ti, hf) for ti in tiles for hf in range(2)]

    def load_ln(self, gname, bname, l):
        s = self.s
        s.dma("sp", "c0", self.lng[:], self.inp[gname][l:l + 1, :].broadcast_to([128, D]), writes=["lng"])
        s.dma("sp", "c1", self.lnb[:], self.inp[bname][l:l + 1, :].broadcast_to([128, D]), writes=["lnb"])

    def epilogue(self, ep, t, halves, hkeys, src_dram, sname, dst_dram, dname, router=False):
        s = self.s
        b, ti = divmod(t, TPB)
        i = ep["n"] % 2
        ep["n"] += 1
        xres, tt, st6, mv, rstd = ep["xres"][i], ep["t"][i], ep["st6"][i], ep["mv"][i], ep["rstd"][i]
        K = lambda n: "ep_%s%d" % (n, i)
        s.dma("sp", self.chan("xl", 3), xres[:], src_dram[t * 128:(t + 1) * 128, :], reads=["dram_%s_%d" % (sname, t)], writes=[K("xres")])
        for hf in range(2):
            s.op("dve", lambda e, hf=hf: e.scalar_tensor_tensor(out=tt[:, hf * 512:(hf + 1) * 512], in0=xres[:, hf * 512:(hf + 1) * 512], scalar=ALPHA,
                                                              in1=halves[hf], op0=ALU.mult, op1=ALU.add),
                 reads=[K("xres"), hkeys[hf]], writes=[K("t%d" % hf)])
            s.op("dve", lambda e, hf=hf: e.bn_stats(out=st6[:, hf, :], in_=tt[:, hf * 512:(hf + 1) * 512]), reads=[K("t%d" % hf)], writes=[K("st%d" % hf)])
        s.op("dve", lambda e: e.bn_aggr(out=mv[:], in_=st6[:].rearrange("p a b -> p (a b)")), reads=[K("st0"), K("st1")], writes=[K("mv")])
        s.op("act", lambda e: e.activation(out=rstd[:], in_=mv[:, 1:2], func=AF.Sqrt, bias=self.epsc[:], scale=1.0), reads=[K("mv"), "epsc"], writes=[K("rstd")])
        s.op("dve", lambda e: e.reciprocal(out=rstd[:], in_=rstd[:]), reads=[K("rstd")], writes=[K("rstd")])
        for hf in range(2):
            sl = slice(hf * 512, (hf + 1) * 512)
            s.op("dve", lambda e, sl=sl: e.tensor_scalar(out=tt[:, sl], in0=tt[:, sl], scalar1=mv[:, 0:1], scalar2=rstd[:, 0:1], op0=ALU.subtract, op1=ALU.mult),
                 reads=[K("t%d" % hf), K("mv"), K("rstd")], writes=[K("t%d" % hf)])
            s.op("pool", lambda e, sl=sl: e.tensor_tensor(out=tt[:, sl], in0=tt[:, sl], in1=self.lng[:, sl], op=ALU.mult), reads=[K("t%d" % hf), "lng"], writes=[K("t%d" % hf)])
            s.op("pool", lambda e, sl=sl: e.tensor_tensor(out=tt[:, sl], in0=tt[:, sl], in1=self.lnb[:, sl], op=ALU.add), reads=[K("t%d" % hf), "lnb"], writes=[K("t%d" % hf)])
        s.dma("sp", self.chan("xs", 3), dst_dram[t * 128:(t + 1) * 128, :], tt[:], reads=[K("t0"), K("t1")], writes=["dram_%s_%d" % (dname, t)])
        tk = [K("t0"), K("t1")]
        if router:
            xTf = ep["xTf"][i]
            self.transpose_to_xT(tt, tk, b, ti, xTf=xTf, xTfkey=K("xTf"))
            self.route(ep, i, t, xTf, [K("xTf0"), K("xTf1")])
        else:
            self.transpose_to_xT(tt, tk, b, ti)

    def ep_alloc(self, st, router=False):
        ep = {"n": 0}
        ep["xres"] = [self.sb(st, "xres", [128, D], F32) for _ in range(2)]
        ep["t"] = [self.sb(st, "tt", [128, D], F32) for _ in range(2)]
        ep["st6"] = [self.sb(st, "st6", [128, 2, 6], F32) for _ in range(2)]
        ep["mv"] = [self.sb(st, "mv", [128, 2], F32) for _ in range(2)]
        ep["rstd"] = [self.sb(st, "rstd", [128, 1], F32) for _ in range(2)]
        if router:
            ep["xTf"] = [self.sb(st, "xTf", [128, 8, 128], F32) for _ in range(2)]
            ep["r"] = [[self.sb(st, "r%d" % j, [128, 16], F32) for j in range(8)] for _ in range(2)]
        return ep

    def route(self, ep, i, t, xTf, xkeys):
        s = self.s
        bk = self.bank()
        pk = "ps%d" % bk
        lg = self.ps[bk][:, 0:16]
        for kc in range(8):
            s.op("pe", lambda e, kc=kc: e.matmul(lg, xTf[:, kc, :], self.rw[:, kc, :], start=(kc == 0), stop=(kc == 7)), reads=xkeys + ["rw"], writes=[pk])
        sc, sel, eq, m, t4, ge, msk, gsel = ep["r"][i]
        R = lambda n: "rt_%s%d" % (n, i)
        v4 = lambda a: a[:].rearrange("p (g k) -> p g k", g=4)
        bc = lambda a, c0: a[:, c0:c0 + 4].unsqueeze(2).broadcast_to([128, 4, 4])
        s.op("act", lambda e: e.activation(out=sc[:], in_=lg, func=AF.Sigmoid), reads=[pk], writes=[R("sc")])
        s.op("dve", lambda e: e.tensor_tensor(out=sel[:], in0=sc[:], in1=self.rb[:], op=ALU.add), reads=[R("sc"), "rb"], writes=[R("sel")])
        s.op("dve", lambda e: e.tensor_reduce(out=m[:, 0:4], in_=v4(sel), axis=AX.X, op=ALU.max), reads=[R("sel")], writes=[R("m")])
        s.op("dve", lambda e: e.tensor_tensor(out=v4(eq), in0=v4(sel), in1=bc(m, 0), op=ALU.is_equal), reads=[R("sel"), R("m")], writes=[R("eq")])
        s.op("dve", lambda e: e.scalar_tensor_tensor(out=eq[:], in0=eq[:], scalar=-1e9, in1=sel[:], op0=ALU.mult, op1=ALU.add), reads=[R("eq"), R("sel")], writes=[R("eq")])
        s.op("dve", lambda e: e.tensor_reduce(out=m[:, 4:8], in_=v4(eq), axis=AX.X, op=ALU.max), reads=[R("eq"), R("m")], writes=[R("m")])
        s.op("dve", lambda e: e.tensor_tensor(out=m[:, 8:12], in0=m[:, 0:4], in1=m[:, 4:8], op=ALU.add), reads=[R("m")], writes=[R("m")])
        s.op("dve", lambda e: e.tensor_reduce(out=m[:, 12:13], in_=m[:, 8:12], axis=AX.X, op=ALU.max), reads=[R("m")], writes=[R("m")])
        s.op("dve", lambda e: e.tensor_scalar(out=t4[:, 0:4], in0=m[:, 8:12], scalar1=m[:, 12:13], scalar2=None, op0=ALU.is_ge), reads=[R("m")], writes=[R("t4")])
        s.op("dve", lambda e: e.tensor_tensor(out=v4(ge), in0=v4(sel), in1=bc(m, 4), op=ALU.is_ge), reads=[R("sel"), R("m")], writes=[R("ge")])
        s.op("dve", lambda e: e.tensor_tensor(out=v4(msk), in0=v4(ge), in1=bc(t4, 0), op=ALU.mult), reads=[R("ge"), R("t4")], writes=[R("msk")])
        s.op("dve", lambda e: e.tensor_tensor(out=gsel[:], in0=sc[:], in1=msk[:], op=ALU.mult), reads=[R("sc"), R("msk")], writes=[R("gsel")])
        s.op("dve", lambda e: e.tensor_reduce(out=m[:, 13:14], in_=gsel[:], axis=AX.X, op=ALU.add), reads=[R("gsel"), R("m")], writes=[R("m")])
        s.op("dve", lambda e: e.reciprocal(out=m[:, 14:15], in_=m[:, 13:14]), reads=[R("m")], writes=[R("m")])
        s.op("dve", lambda e: e.tensor_scalar(out=self.wgt[:, t, :], in0=gsel[:], scalar1=m[:, 14:15], scalar2=None, op0=ALU.mult), reads=[R("gsel"), R("m")], writes=["wgt%d" % t])

    def mm_acc(self, out, pk, lhs_fn, rhs_fn, nk, reads):
        s = self.s
        for kc in range(nk):
            s.op("pe", lambda e, kc=kc: e.matmul(out, lhs_fn(kc), rhs_fn(kc), start=(kc == 0), stop=(kc == nk - 1)), reads=reads, writes=[pk])

    def dma_nc(self, eng, chan, out, in_, reads=(), writes=()):
        nc = self.nc
        op = self.s.dma(eng, chan, out, in_, reads=reads, writes=writes)

        def fn(e):
            with nc.allow_non_contiguous_dma(reason="tiny one-time parameter load"):
                return e.dma_start(out=out, in_=in_)
        op.fn = fn
        return op

    def mixer(self, l, src, sname, dst, dname):
        import contextlib
        s, nc = self.s, self.nc
        with contextlib.ExitStack() as st:
            SB = lambda n, shp, dt: self.sb(st, n, shp, dt)
            w_in = SB("w_in", [128, 8, N_IN], BF16)
            w_mo = SB("w_mo", [128, 8, D], BF16)
            pw = SB("pw", [128, 4, 128], BF16)
            cvw = SB("cvw", [128, 4, 8], F32)
            psc = SB("psc", [128, 4], F32)
            hng = SB("hng", [128, M], F32)
            bif = SB("bif", [128, 8], F32)
            wsrc = self.inp["w_in"][l].rearrange("(kc p) n -> p kc n", p=128)
            groups = [(0, 512), (512, 1024), (1024, 1536), (1536, 2048), (2048, N_IN)]
            for gi, (a, b_) in enumerate(groups):
                s.dma("pool", self.chan("wl", 4), w_in[:, :, a:b_], wsrc[:, :, a:b_], writes=["w_in%d" % gi])
            msrc = self.inp["w_mix_out"][l].rearrange("(kc p) n -> p kc n", p=128)
            for hf in range(2):
                s.dma("pool", self.chan("wl", 4), w_mo[:, :, hf * 512:(hf + 1) * 512], msrc[:, :, hf * 512:(hf + 1) * 512], writes=["w_mo%d" % hf])
            s.dma("pool", self.chan("wl", 4), pw[:], self.inp["pool_w"][l].rearrange("g c d -> c g d"), writes=["pw"])
            for j4 in range(4):
                self.dma_nc("sp", "c0", cvw[:, j4, :], self.inp["conv_qk"][l, j4].rearrange("(k c) -> c k", c=128), writes=["cvw"])
            self.dma_nc("sp", "c1", psc[:], self.inp["pool_scale"][l].rearrange("(g c) -> c g", c=128), writes=["psc"])
            s.dma("sp", "c0", hng[:], self.inp["head_norm_g"][l:l + 1, :].broadcast_to([128, M]), writes=["hng"])
            s.dma("sp", "c1", bif[:, 0:4], self.inp["b_i"][l:l + 1, :].broadcast_to([128, 4]), writes=["bif"])
            s.dma("sp", "c0", bif[:, 4:8], self.inp["b_f"][l:l + 1, :].broadcast_to([128, 4]), writes=["bif"])
            self.load_ln("ln_mix_g", "ln_mix_b", l)

            zq = SB("zq", [128, 515], F32)
            zh = SB("zh", [128, 8, 3], F32)
            acc = SB("acc", [128, 512], F32)
            qkT = SB("qkT", [128, 8, 512], BF16)
            ktok = SB("ktok", [128, 4, 512], BF16)
            vext = SB("vext", [128, 4, 4, 130], BF16)
            vsc = SB("vsc", [128, 4, 130], BF16)
            og = SB("og", [128, 4, 512], BF16)
            gts = [SB("gt", [128, 32], F32) for _ in range(4)]
            pT = SB("pT", [128, 4, 128], BF16)
            hraw = SB("hraw", [128, 512], F32)
            hst = SB("hst", [128, 4, 6], F32)
            hmv = SB("hmv", [128, 4, 2], F32)
            hrs = SB("hrs", [128, 4], F32)
            hb = SB("hb", [128, 512], BF16)
            Cn = SB("Cn", [128, 4, 130], F32)
            Cnb = SB("Cnb", [128, 4, 130], BF16)
            ub = SB("ub", [128, 528], F32)
            uh = SB("uh", [128, 4, 16], F32)
            sA = SB("sA", [128, 528], F32)
            sB_ = SB("sB", [128, 528], F32)
            pooledT = SB("pooledT", [128, 4, 512], BF16)
            mixedT = SB("mixedT", [128, 8, 512], BF16)
            ep = self.ep_alloc(st)
            s.op("pool", lambda e: e.memset(vext[:], 1.0), writes=["vext%d" % i for i in range(4)])

            for b in range(NB):
                s.op("pool", lambda e: e.memset(Cn[:], 0.0), writes=["Cn"])
                s.op("pool", lambda e: e.memset(Cnb[:], 0.0), writes=["Cnb"])
                s.op("pool", lambda e: e.memset(zh[:], 0.0), writes=["zh%d" % c for c in range(8)])
                s.op("pool", lambda e: e.memset(uh[:], 0.0), writes=["uh%d" % g for g in range(4)])
                for j in range(4):
                    tok0 = j * 512
                    xk = self.xT_keys(b, range(4 * j, 4 * j + 4))
                    rhs_blk = lambda kc, b=b, tok0=tok0: self.xT[:, b, kc, tok0:tok0 + 512]
                    for c in range(8):
                        bk = self.bank()
                        pk = "ps%d" % bk
                        pz = self.ps[bk]
                        self.mm_acc(pz[:], pk, lambda kc, c=c: w_in[:, kc, c * 128:(c + 1) * 128], rhs_blk, 8, xk + ["w_in%d" % (c // 4)])
                        s.op("act", lambda e, pz=pz: e.copy(zq[:, 3:515], pz[:]), reads=[pk], writes=["zq"])
                        s.op("dve", lambda e, c=c: e.tensor_copy(out=zq[:, 0:3], in_=zh[:, c, :]), reads=["zh%d" % c], writes=["zq"])
                        s.op("dve", lambda e, c=c: e.tensor_scalar(out=acc[:], in0=zq[:, 0:512], scalar1=cvw[:, 0, c:c + 1], scalar2=None, op0=ALU.mult),
                             reads=["zq", "cvw"], writes=["acc"])
                        for j2 in range(1, 4):
                            s.op("dve", lambda e, c=c, j2=j2: e.scalar_tensor_tensor(out=acc[:], in0=zq[:, j2:j2 + 512], scalar=cvw[:, j2, c:c + 1], in1=acc[:],
                                                                                    op0=ALU.mult, op1=ALU.add), reads=["zq", "cvw", "acc"], writes=["acc"])
                        s.op("pool", lambda e, c=c: e.tensor_copy(out=zh[:, c, :], in_=zq[:, 512:515]), reads=["zq"], writes=["zh%d" % c])
                        s.op("act", lambda e, c=c: e.activation(out=qkT[:, c, :], in_=acc[:], func=AF.Silu), reads=["acc"], writes=["qkT%d" % c])
                    for g in range(4):
                        bk = self.bank()
                        pk = "ps%d" % bk
                        pz = self.ps[bk]
                        col0 = 2056 + g * 128
                        self.mm_acc(pz[:], pk, lambda kc, col0=col0: w_in[:, kc, col0:col0 + 128], rhs_blk, 8, xk + ["w_in4"])
                        s.op("act", lambda e, pz=pz: e.copy(ub[:, 16:528], pz[:]), reads=[pk], writes=["ub"])
                        s.op("dve", lambda e, g=g: e.tensor_copy(out=ub[:, 0:16], in_=uh[:, g, :]), reads=["uh%d" % g], writes=["ub"])
                        cur, curk = ub, "ub"
                        lo = 0
                        for step in range(g + 1):
                            sh = 2 ** step
                            nxt, nxtk = (sA, "sA") if step % 2 == 0 else (sB_, "sB")
                            lo2 = lo + sh
                            s.op("pool" if step % 2 else "dve", lambda e, cur=cur, nxt=nxt, lo2=lo2, sh=sh: e.tensor_tensor(out=nxt[:, lo2:528], in0=cur[:, lo2:528], in1=cur[:, lo2 - sh:528 - sh], op=ALU.add),
                                 reads=[curk], writes=[nxtk])
                            cur, curk, lo = nxt, nxtk, lo2
                        w = float(2 ** (g + 1))
                        s.op("dve", lambda e, cur=cur, g=g, w=w: e.scalar_tensor_tensor(out=pooledT[:, g, :], in0=cur[:, 16:528], scalar=1.0 / w, in1=ub[:, 16:528],
                                                                                      op0=ALU.mult, op1=ALU.subtract), reads=[curk, "ub"], writes=["pooledT%d" % g])
                        if j == 0:
                            oth, othk = (sB_, "sB") if cur is sA else (sA, "sA")
                            s.op("dve", lambda e, cur=cur, oth=oth, g=g: e.tensor_tensor(out=oth[:, 0:16], in0=cur[:, 16:32], in1=self.rdiv[:, g, :], op=ALU.mult),
                                 reads=[curk, "rdiv%d" % g], writes=[othk])
                            s.op("dve", lambda e, oth=oth, g=g: e.tensor_tensor(out=pooledT[:, g, 0:16], in0=oth[:, 0:16], in1=ub[:, 16:32], op=ALU.subtract),
                                 reads=[othk, "ub"], writes=["pooledT%d" % g])
                        s.op("pool", lambda e, g=g: e.tensor_copy(out=uh[:, g, :], in_=ub[:, 512:528]), reads=["ub"], writes=["uh%d" % g])
                        bk2 = self.bank()
                        pk2 = "ps%d" % bk2
                        pz2 = self.ps[bk2]
                        s.op("pe", lambda e, pz2=pz2, g=g: e.matmul(pz2[:], pw[:, g, :], pooledT[:, g, :], start=True, stop=True), reads=["pw", "pooledT%d" % g], writes=[pk2])
                        s.op("dve", lambda e, pz2=pz2, g=g: e.tensor_scalar(out=mixedT[:, 4 + g, :], in0=pz2[:], scalar1=psc[:, g:g + 1], scalar2=None, op0=ALU.mult),
                             reads=[pk2, "psc"], writes=["mxp%d" % g])
                    for i in range(4):
                        ti = 4 * j + i
                        xki = self.xT_keys(b, [ti])
                        lhs_t = lambda kc, b=b, ti=ti: self.xT[:, b, kc, ti * 128:(ti + 1) * 128]
                        bk = self.bank(); pk = "ps%d" % bk; pz = self.ps[bk]
                        self.mm_acc(pz[:], pk, lhs_t, lambda kc: w_in[:, kc, 1024:1536], 8, xki + ["w_in2"])
                        s.op("act", lambda e, pz=pz, i=i: e.copy(vext[:, i, :, 0:128], pz[:].rearrange("p (h e) -> p h e", h=4)), reads=[pk], writes=["vext%d" % i])
                        bk = self.bank(); pk = "ps%d" % bk; pz = self.ps[bk]
                        self.mm_acc(pz[:], pk, lhs_t, lambda kc: w_in[:, kc, 1536:2048], 8, xki + ["w_in3"])
                        s.op("act", lambda e, pz=pz, i=i: e.activation(out=og[:, i, :], in_=pz[:], func=AF.Sigmoid), reads=[pk], writes=["og%d" % i])
                        bk = self.bank(); pk = "ps%d" % bk; pz = self.ps[bk]
                        self.mm_acc(pz[:, 0:8], pk, lhs_t, lambda kc: w_in[:, kc, 2048:2056], 8, xki + ["w_in4"])
                        gt = gts[i]
                        G = "G%d" % i
                        s.op("dve", lambda e, pz=pz, gt=gt: e.tensor_tensor(out=gt[:, 0:8], in0=pz[:, 0:8], in1=bif[:], op=ALU.add), reads=[pk, "bif"], writes=[G])
                        s.op("act", lambda e, gt=gt: e.activation(out=gt[:, 8:12], in_=gt[:, 4:8], func=AF.Exp, scale=-1.0), reads=[G], writes=[G])
                        s.op("act", lambda e, gt=gt: e.activation(out=gt[:, 8:12], in_=gt[:, 8:12], func=AF.Ln, bias=1.0), reads=[G], writes=[G])
                        bk = self.bank(); pkc = "ps%d" % bk; pc = self.ps[bk]
                        s.op("pe", lambda e, pc=pc, gt=gt: e.matmul(pc[:, 0:4], self.triu[:], gt[:, 8:12], start=True, stop=True), reads=[G, "triu"], writes=[pkc])
                        s.op("pe", lambda e, pc=pc, gt=gt: e.matmul(pc[:, 4:8], self.ones_f[:], gt[:, 8:12], start=True, stop=True), reads=[G, "ones_f"], writes=[pkc])
                        s.op("dve", lambda e, pc=pc, gt=gt: e.tensor_tensor(out=gt[:, 12:16], in0=gt[:, 0:4], in1=pc[:, 0:4], op=ALU.add), reads=[G, pkc], writes=[G])
                        s.op("act", lambda e, gt=gt: e.activation(out=gt[:, 12:16], in_=gt[:, 12:16], func=AF.Exp), reads=[G], writes=[G])
                        s.op("act", lambda e, pc=pc, gt=gt: e.activation(out=gt[:, 16:24], in_=pc[:, 0:8], func=AF.Exp, scale=-1.0), reads=[G, pkc], writes=[G])
                        bk = self.bank(); pk = "ps%d" % bk; pzb = self.psb[bk]
                        for h in range(4):
                            s.op("pe", lambda e, pzb=pzb, h=h, i=i: e.transpose(pzb[:, h * 128:(h + 1) * 128], qkT[:, 4 + h, i * 128:(i + 1) * 128], self.ident_b[:]),
                                 reads=["qkT%d" % (4 + h), "ident_b"], writes=[pk])
                        s.op("act", lambda e, pzb=pzb, i=i: e.copy(ktok[:, i, :], pzb[:, 0:512]), reads=[pk], writes=["ktok%d" % i])
                    for i in range(4):
                        ti = 4 * j + i
                        t = b * TPB + ti
                        gt = gts[i]
                        G = "G%d" % i
                        cols = slice(i * 128, (i + 1) * 128)
                        bk = self.bank(); pkS = "ps%d" % bk; pS = self.ps[bk]
                        for h in range(4):
                            s.op("pe", lambda e, pS=pS, h=h, cols=cols: e.matmul(pS[:, h * 128:(h + 1) * 128], qkT[:, 4 + h, cols], qkT[:, h, cols], start=True, stop=True),
                                 reads=["qkT%d" % h, "qkT%d" % (4 + h)], writes=[pkS])
                        for h in range(4):
                            s.op("dve", lambda e, pS=pS, h=h, gt=gt: e.scalar_tensor_tensor(out=pT[:, h, :], in0=pS[:, h * 128:(h + 1) * 128], scalar=gt[:, 12 + h:13 + h], in1=self.maskc[:],
                                                                                       op0=ALU.mult, op1=ALU.mult), reads=[pkS, G, "maskc"], writes=["pT%d" % h])
                            s.op("pool", lambda e, h=h, i=i, gt=gt: e.tensor_scalar(out=vsc[:, h, 0:129], in0=vext[:, i, h, 0:129], scalar1=gt[:, 12 + h:13 + h], scalar2=None, op0=ALU.mult),
                                 reads=["vext%d" % i, G], writes=["vsc%d" % h])
                        pO, pkO, pSt, pkSt = [], [], [], []
                        for bb in range(2):
                            bk = self.bank(); pkO.append("ps%d" % bk); pO.append(self.ps[bk][:, 0:260].rearrange("p (h e) -> p h e", h=2))
                        for bb in range(2):
                            bk = self.bank(); pkSt.append("ps%d" % bk); pSt.append(self.ps[bk][:, 0:260].rearrange("p (h e) -> p h e", h=2))
                        for h in range(4):
                            o_ap = pO[h // 2][:, h % 2, 0:129]
                            s.op("pe", lambda e, o_ap=o_ap, h=h, i=i: e.matmul(o_ap, pT[:, h, :], vext[:, i, h, 0:129], start=True, stop=False),
                                 reads=["pT%d" % h, "vext%d" % i], writes=[pkO[h // 2]])
                            s.op("pe", lambda e, o_ap=o_ap, h=h, cols=cols: e.matmul(o_ap, qkT[:, h, cols], Cnb[:, h, 0:129], start=False, stop=True),
                                 reads=["qkT%d" % h, "Cnb"], writes=[pkO[h // 2]])
                        for h in range(4):
                            st_ap = pSt[h // 2][:, h % 2, 0:129]
                            s.op("pe", lambda e, st_ap=st_ap, h=h, i=i: e.matmul(st_ap, ktok[:, i, h * 128:(h + 1) * 128], vsc[:, h, 0:129], start=True, stop=True),
                                 reads=["ktok%d" % i, "vsc%d" % h], writes=[pkSt[h // 2]])
                        for bb in range(2):
                            s.op("dve", lambda e, bb=bb, gt=gt, po=pO[bb]: e.tensor_tensor(out=gt[:, 24 + 2 * bb:26 + 2 * bb], in0=po[:, :, 128], in1=gt[:, 16 + 2 * bb:18 + 2 * bb], op=ALU.mult),
                                 reads=[pkO[bb], G], writes=[G])
                        s.op("dve", lambda e, gt=gt: e.tensor_scalar(out=gt[:, 28:32], in0=gt[:, 24:28], scalar1=1.0, scalar2=None, op0=ALU.max), reads=[G], writes=[G])
                        s.op("dve", lambda e, gt=gt: e.scalar_tensor_tensor(out=gt[:, 24:28], in0=gt[:, 24:28], scalar=-1.0, in1=gt[:, 28:32], op0=ALU.mult, op1=ALU.max), reads=[G], writes=[G])
                        s.op("dve", lambda e, gt=gt: e.reciprocal(out=gt[:, 24:28], in_=gt[:, 24:28]), reads=[G], writes=[G])
                        s.op("dve", lambda e, gt=gt: e.tensor_tensor(out=gt[:, 28:32], in0=gt[:, 24:28], in1=gt[:, 16:20], op=ALU.mult), reads=[G], writes=[G])
                        for h in range(4):
                            s.op("act", lambda e, h=h, gt=gt, po=pO[h // 2]: e.activation(out=hraw[:, h * 128:(h + 1) * 128], in_=po[:, h % 2, 0:128], func=AF.Copy, scale=gt[:, 28 + h:29 + h]),
                                 reads=[pkO[h // 2], G], writes=["hraw%d" % h])
                        for h in range(4):
                            s.op("dve", lambda e, h=h: e.bn_stats(out=hst[:, h, :], in_=hraw[:, h * 128:(h + 1) * 128]), reads=["hraw%d" % h], writes=["hst%d" % h])
                            s.op("dve", lambda e, h=h: e.bn_aggr(out=hmv[:, h, :], in_=hst[:, h, :]), reads=["hst%d" % h], writes=["hmv%d" % h])
                        hmk = ["hmv%d" % h for h in range(4)]
                        s.op("act", lambda e: e.activation(out=hrs[:], in_=hmv[:, :, 1], func=AF.Sqrt, bias=self.epsc[:], scale=1.0), reads=hmk + ["epsc"], writes=["hrs"])
                        s.op("dve", lambda e: e.reciprocal(out=hrs[:], in_=hrs[:]), reads=["hrs"], writes=["hrs"])
                        for h in range(4):
                            s.op("dve", lambda e, h=h: e.tensor_scalar(out=hraw[:, h * 128:(h + 1) * 128], in0=hraw[:, h * 128:(h + 1) * 128], scalar1=hmv[:, h, 0:1], scalar2=hrs[:, h:h + 1],
                                                                     op0=ALU.subtract, op1=ALU.mult), reads=["hraw%d" % h, "hmv%d" % h, "hrs"], writes=["hraw%d" % h])
                        hk = ["hraw%d" % h for h in range(4)]
                        s.op("dve", lambda e: e.tensor_tensor(out=hraw[:], in0=hraw[:], in1=hng[:], op=ALU.mult), reads=hk + ["hng"], writes=hk)
                        s.op("pool", lambda e, i=i: e.tensor_tensor(out=hb[:], in0=hraw[:], in1=og[:, i, :], op=ALU.mult), reads=hk + ["og%d" % i], writes=["hb"])
                        bk = self.bank(); pk = "ps%d" % bk; pzb = self.psb[bk]
                        for h in range(4):
                            s.op("pe", lambda e, pzb=pzb, h=h: e.transpose(pzb[:, h * 128:(h + 1) * 128], hb[:, h * 128:(h + 1) * 128], self.ident_b[:]), reads=["hb", "ident_b"], writes=[pk])
                        s.op("act", lambda e, pzb=pzb, cols=cols: e.copy(mixedT[:, 0:4, cols], pzb[:, 0:512].rearrange("p (h t) -> p h t", h=4)), reads=[pk], writes=["mxh%d" % i])
                        for bb in range(2):
                            s.op("dve", lambda e, bb=bb, pst=pSt[bb]: e.tensor_tensor(out=Cn[:, 2 * bb:2 * bb + 2, 0:129], in0=pst[:, :, 0:129], in1=Cn[:, 2 * bb:2 * bb + 2, 0:129], op=ALU.add),
                                 reads=[pkSt[bb], "Cn"], writes=["Cn"])
                        for h in range(4):
                            s.op("pool", lambda e, h=h, gt=gt: e.tensor_scalar(out=Cnb[:, h, 0:129], in0=Cn[:, h, 0:129], scalar1=gt[:, 20 + h:21 + h], scalar2=QSCALE, op0=ALU.mult, op1=ALU.mult),
                                 reads=["Cn", G], writes=["Cnb"])
                        for h in range(4):
                            s.op("pool", lambda e, h=h, gt=gt: e.tensor_scalar(out=Cn[:, h, 0:129], in0=Cn[:, h, 0:129], scalar1=gt[:, 20 + h:21 + h], scalar2=None, op0=ALU.mult),
                                 reads=["Cn", G], writes=["Cn"])
                        halves, hkeys = [], []
                        for hf in range(2):
                            bk = self.bank(); pk = "ps%d" % bk; pz = self.ps[bk]
                            self.mm_acc(pz[:], pk, lambda kc, cols=cols: mixedT[:, kc, cols], lambda kc, hf=hf: w_mo[:, kc, hf * 512:(hf + 1) * 512], 8,
                                        ["mxh%d" % i] + ["mxp%d" % g for g in range(4)] + ["w_mo%d" % hf])
                            halves.append(pz[:]); hkeys.append(pk)
                        self.epilogue(ep, t, halves, hkeys, src, sname, dst, dname)
            self.barrier()

    def xattn(self, l, src, sname, dst, dname):
        import contextlib
        s, nc = self.s, self.nc
        with contextlib.ExitStack() as st:
            SB = lambda n, shp, dt: self.sb(st, n, shp, dt)
            wq = SB("wq", [128, 8, D], BF16)
            wo = SB("wo", [128, 8, D], BF16)
            wkv = SB("wkv", [128, 8, 2 * D], BF16)
            for nm, wt, key, ncol in (("w_xkv", wkv, "wkv", 2048), ("w_xq", wq, "wq", 1024), ("w_xo", wo, "wo", 1024)):
                wsrc = self.inp[nm][l].rearrange("(kc p) n -> p kc n", p=128)
                for cg in range(ncol // 512):
                    s.dma("pool", self.chan("wl", 4), wt[:, :, cg * 512:(cg + 1) * 512], wsrc[:, :, cg * 512:(cg + 1) * 512], writes=["%s%d" % (key, cg)])
            self.load_ln("ln_x_g", "ln_x_b", l)
            memin = SB("memin", [128, D], F32)
            memT = SB("memT", [128, 8, MEM], BF16)
            KT = SB("KT", [128, 8, MEM], BF16)
            V = SB("V", [128, 2, D], BF16)
            qT = SB("qT", [128, 8, 512], BF16)
            P = SB("P", [128, 4, MEM], F32)
            Pn = SB("Pn", [128, 4, MEM], BF16)
            PT = SB("PT", [128, 8, 128], BF16)
            oT = SB("oT", [128, 8, 128], BF16)
            sm = SB("sm", [128, 12], F32)
            ep = self.ep_alloc(st, router=True)
            for b in range(NB):
                for mt in range(2):
                    s.dma("sp", self.chan("xl", 3), memin[:], self.inp["mem"][b * MEM + mt * 128: b * MEM + (mt + 1) * 128, :], writes=["memin"])
                    for hf in range(2):
                        bk = self.bank(); pk = "ps%d" % bk; pz = self.ps[bk]
                        for c in range(4):
                            kc = hf * 4 + c
                            s.op("pe", lambda e, pz=pz, c=c, kc=kc: e.transpose(pz[:, c * 128:(c + 1) * 128], memin[:, kc * 128:(kc + 1) * 128], self.ident_f[:]),
                                 reads=["memin", "ident_f"], writes=[pk])
                        s.op("act", lambda e, pz=pz, hf=hf, mt=mt: e.copy(memT[:, hf * 4:(hf + 1) * 4, mt * 128:(mt + 1) * 128], pz[:].rearrange("p (c t) -> p c t", c=4)),
                             reads=[pk], writes=["memT"])
                for c in range(8):
                    bk = self.bank(); pk = "ps%d" % bk; pz = self.ps[bk]
                    self.mm_acc(pz[:, 0:MEM], pk, lambda kc, c=c: wkv[:, kc, c * 128:(c + 1) * 128], lambda kc: memT[:, kc, :], 8, ["memT", "wkv%d" % (c // 4)])
                    s.op("act", lambda e, pz=pz, c=c: e.mul(KT[:, c, :], pz[:, 0:MEM], XSCALE), reads=[pk], writes=["KT"])
                for mt in range(2):
                    for hf in range(2):
                        bk = self.bank(); pk = "ps%d" % bk; pz = self.ps[bk]
                        self.mm_acc(pz[:], pk, lambda kc, mt=mt: memT[:, kc, mt * 128:(mt + 1) * 128], lambda kc, hf=hf: wkv[:, kc, 1024 + hf * 512:1024 + (hf + 1) * 512], 8,
                                    ["memT", "wkv%d" % (2 + hf)])
                        s.op("dve", lambda e, pz=pz, mt=mt, hf=hf: e.tensor_copy(out=V[:, mt, hf * 512:(hf + 1) * 512], in_=pz[:]), reads=[pk], writes=["V"])
                for j in range(4):
                    tok0 = j * 512
                    xk = self.xT_keys(b, range(4 * j, 4 * j + 4))
                    for c in range(8):
                        bk = self.bank(); pk = "ps%d" % bk; pz = self.ps[bk]
                        self.mm_acc(pz[:], pk, lambda kc, c=c: wq[:, kc, c * 128:(c + 1) * 128], lambda kc, b=b, tok0=tok0: self.xT[:, b, kc, tok0:tok0 + 512], 8, xk + ["wq%d" % (c // 4)])
                        s.op("act" if c % 2 else "dve", (lambda e, pz=pz, c=c: e.copy(qT[:, c, :], pz[:])) if c % 2 else (lambda e, pz=pz, c=c: e.tensor_copy(out=qT[:, c, :], in_=pz[:])),
                             reads=[pk], writes=["qT%d" % c])
                    for i in range(4):
                        ti = 4 * j + i
                        t = b * TPB + ti
                        cols = slice(i * 128, (i + 1) * 128)
                        pS, pkS = [], []
                        for bb in range(2):
                            bk = self.bank(); pkS.append("ps%d" % bk); pS.append(self.ps[bk][:].rearrange("p (h m) -> p h m", h=2))
                        for h in range(4):
                            for hh in range(2):
                                s.op("pe", lambda e, h=h, hh=hh, cols=cols, o_ap=pS[h // 2][:, h % 2, :]: e.matmul(o_ap, qT[:, 2 * h + hh, cols], KT[:, 2 * h + hh, :], start=(hh == 0), stop=(hh == 1)),
                                     reads=["qT%d" % (2 * h + hh), "KT"], writes=[pkS[h // 2]])
                        for bb in range(2):
                            s.op("dve", lambda e, bb=bb, ps_=pS[bb]: e.tensor_reduce(out=sm[:, 2 * bb:2 * bb + 2], in_=ps_, axis=AX.X, op=ALU.max, negate=True), reads=[pkS[bb]], writes=["sm_mx"])
                        for h in range(4):
                            s.op("act", lambda e, h=h, ps_=pS[h // 2]: e.activation(out=P[:, h, :], in_=ps_[:, h % 2, :], func=AF.Exp, bias=sm[:, h:h + 1], scale=1.0, accum_out=sm[:, 4 + h:5 + h]),
                                 reads=[pkS[h // 2], "sm_mx"], writes=["P%d" % h, "sm_s%d" % h])
                        s.op("dve", lambda e: e.reciprocal(out=sm[:, 8:12], in_=sm[:, 4:8]), reads=["sm_s%d" % h for h in range(4)], writes=["sm_r"])
                        for h in range(4):
                            s.op("pool" if h % 2 else "dve", lambda e, h=h: e.tensor_scalar(out=Pn[:, h, :], in0=P[:, h, :], scalar1=sm[:, 8 + h:9 + h], scalar2=None, op0=ALU.mult),
                                 reads=["P%d" % h, "sm_r"], writes=["Pn%d" % h])
                        bk = self.bank(); pk = "ps%d" % bk; pzb = self.psb[bk]
                        for h in range(4):
                            for mc in range(2):
                                s.op("pe", lambda e, pzb=pzb, h=h, mc=mc: e.transpose(pzb[:, (2 * h + mc) * 128:(2 * h + mc + 1) * 128], Pn[:, h, mc * 128:(mc + 1) * 128], self.ident_b[:]),
                                     reads=["Pn%d" % h, "ident_b"], writes=[pk])
                        s.op("act", lambda e, pzb=pzb: e.copy(PT[:], pzb[:].rearrange("p (c t) -> p c t", c=8)), reads=[pk], writes=["PT"])
                        pks = []
                        for hf in range(2):
                            bk = self.bank(); pk = "ps%d" % bk; pz = self.ps[bk]; pks.append(pk)
                            for c in range(4):
                                cc = hf * 4 + c
                                h, hh = divmod(cc, 2)
                                for mc in range(2):
                                    s.op("pe", lambda e, pz=pz, c=c, h=h, hh=hh, mc=mc: e.matmul(pz[:, c * 128:(c + 1) * 128], V[:, mc, h * 256 + hh * 128:h * 256 + (hh + 1) * 128], PT[:, 2 * h + mc, :],
                                                                                            start=(mc == 0), stop=(mc == 1)), reads=["V", "PT"], writes=[pk])
                            s.op("dve" if hf else "act", (lambda e, pz=pz, hf=hf: e.tensor_copy(out=oT[:, hf * 4:(hf + 1) * 4, :], in_=pz[:].rearrange("p (c t) -> p c t", c=4))) if hf else
                                 (lambda e, pz=pz, hf=hf: e.copy(oT[:, hf * 4:(hf + 1) * 4, :], pz[:].rearrange("p (c t) -> p c t", c=4))), reads=[pk], writes=["oT%d" % hf])
                        halves, hkeys = [], []
                        for hf in range(2):
                            bk = self.bank(); pk = "ps%d" % bk; pz = self.ps[bk]
                            self.mm_acc(pz[:], pk, lambda kc: oT[:, kc, :], lambda kc, hf=hf: wo[:, kc, hf * 512:(hf + 1) * 512], 8, ["oT0", "oT1", "wo%d" % hf])
                            halves.append(pz[:]); hkeys.append(pk)
                        self.epilogue(ep, t, halves, hkeys, src, sname, dst, dname, router=True)
            self.barrier()

    def moe(self, l, src, sname, dst, dname):
        import contextlib
        s, nc = self.s, self.nc
        with contextlib.ExitStack() as st:
            SB = lambda n, shp, dt: self.sb(st, n, shp, dt)
            yacc = SB("yacc", [128, TPB, D], F32)
            wg = [SB("wg", [128, 8, DE], BF16) for _ in range(2)]
            wu = [SB("wu", [128, 8, DE], BF16) for _ in range(2)]
            wd = [SB("wd", [128, 2, D], BF16) for _ in range(2)]
            sg = [SB("sg", [128, 512], F32) for _ in range(2)]
            hT = [SB("hT", [128, 2, 512], BF16) for _ in range(2)]
            ep = self.ep_alloc(st)
            self.load_ln("ln_moe_g", "ln_moe_b", l)
            n = 0
            for b in range(NB):
                for ex in range(E):
                    r = n % 2
                    n += 1
                    s.dma("pool", "wg%d" % r, wg[r][:], self.inp["w_gate"][l, ex].rearrange("(kc p) n -> p kc n", p=128), writes=["wg%d" % r])
                    s.dma("pool", "wu%d" % r, wu[r][:], self.inp["w_up"][l, ex].rearrange("(kc p) n -> p kc n", p=128), writes=["wu%d" % r])
                    s.dma("pool", "wd%d" % r, wd[r][:], self.inp["w_down"][l, ex].rearrange("(kc p) n -> p kc n", p=128), writes=["wd%d" % r])
                    hn = 0
                    for j in range(4):
                        tok0 = j * 512
                        xk = self.xT_keys(b, range(4 * j, 4 * j + 4))
                        hr = hn % 2
                        hn += 1
                        for c in range(2):
                            bkg = self.bank(); pkg = "ps%d" % bkg; pg = self.ps[bkg]
                            self.mm_acc(pg[:], pkg, lambda kc, c=c, r=r: wg[r][:, kc, c * 128:(c + 1) * 128], lambda kc, b=b, tok0=tok0: self.xT[:, b, kc, tok0:tok0 + 512], 8, xk + ["wg%d" % r])
                            bku = self.bank(); pku = "ps%d" % bku; pu = self.ps[bku]
                            self.mm_acc(pu[:], pku, lambda kc, c=c, r=r: wu[r][:, kc, c * 128:(c + 1) * 128], lambda kc, b=b, tok0=tok0: self.xT[:, b, kc, tok0:tok0 + 512], 8, xk + ["wu%d" % r])
                            s.op("act", lambda e, pg=pg, c=c: e.activation(out=sg[c][:], in_=pg[:], func=AF.Silu), reads=[pkg], writes=["sg%d" % c])
                            s.op("dve", lambda e, pu=pu, c=c, hr=hr: e.tensor_tensor(out=hT[hr][:, c, :], in0=sg[c][:], in1=pu[:], op=ALU.mult), reads=["sg%d" % c, pku], writes=["hT%d_%d" % (hr, c)])
                        for i in range(4):
                            ti = 4 * j + i
                            t = b * TPB + ti
                            cols = slice(i * 128, (i + 1) * 128)
                            for hf in range(2):
                                bk = self.bank(); pk = "ps%d" % bk; pz = self.ps[bk]
                                for c in range(2):
                                    s.op("pe", lambda e, pz=pz, c=c, hr=hr, r=r, hf=hf, cols=cols: e.matmul(pz[:], hT[hr][:, c, cols], wd[r][:, c, hf * 512:(hf + 1) * 512], start=(c == 0), stop=(c == 1)),
                                         reads=["hT%d_%d" % (hr, c), "wd%d" % r], writes=[pk])
                                ya = yacc[:, ti, hf * 512:(hf + 1) * 512]
                                yk = "y%d_%d" % (ti, hf)
                                wcol = self.wgt[:, t, ex:ex + 1]
                                if ex == 0:
                                    s.op("dve", lambda e, pz=pz, ya=ya, wcol=wcol: e.tensor_scalar(out=ya, in0=pz[:], scalar1=wcol, scalar2=None, op0=ALU.mult), reads=[pk, "wgt%d" % t], writes=[yk])
                                else:
                                    s.op("dve", lambda e, pz=pz, ya=ya, wcol=wcol: e.scalar_tensor_tensor(out=ya, in0=pz[:], scalar=wcol, in1=ya, op0=ALU.mult, op1=ALU.add),
                                         reads=[pk, "wgt%d" % t, yk], writes=[yk])
                for ti in range(TPB):
                    t = b * TPB + ti
                    self.epilogue(ep, t, [yacc[:, ti, 0:512], yacc[:, ti, 512:1024]], ["y%d_0" % ti, "y%d_1" % ti], src, sname, dst, dname)
            self.barrier()

    def build(self):
        self.setup()
        self.load_x()
        cur, curname = self.inp["x"], "x"
        stages = []
        for l in range(self.nlayers):
            stages += [("mixer", l), ("xattn", l), ("moe", l)]
        if self.stop_after is not None:
            stages = stages[:self.stop_after]
        for si, (kind, l) in enumerate(stages):
            last = si == len(stages) - 1
            if last:
                dst, dname = self.out, "out"
            else:
                dname = "%s%d" % (kind, l)
                dst = self.scratch(dname)
            self.is_last_stage = last
            getattr(self, kind)(l, cur, curname, dst, dname)
            cur, curname = dst, dname
        s = self.s
        op = s.op("sp", lambda e: e.nop())
        op.deps = list(s.chan_last.values())
        counts = s.emit()
        return counts


class Prog2(Prog):
    def setup(self):
        nc, s = self.nc, self.s
        A = nc.alloc_sbuf_tensor
        self.ps = [nc.alloc_psum_tensor("psb%d" % i, [128, 512], F32) for i in range(8)]
        self.psb = [p.bitcast(BF16) for p in self.ps]
        self.xTd = nc.dram_tensor("xTd", [NT, 128, 8, 128], BF16, kind="Internal").ap()
        self.xTs = [A("xTs%d" % i, [128, 8, 128], BF16) for i in range(2)]
        self.xTs_n = 0
        self.ident_f = A("ident_f", [128, 128], F32)
        self.ident_b = A("ident_b", [128, 128], BF16)
        self.triu = A("triu", [128, 128], F32)
        self.maskc = A("maskc", [128, 128], F32)
        self.ones_f = A("ones_f", [128, 128], F32)
        self.wgt = A("wgt", [128, NT, 16], F32)
        self.rw = A("rw", [128, 8, 16], F32)
        self.rb = A("rb", [128, 16], F32)
        self.rdiv = A("rdiv", [128, 4, 16], F32)
        self.iota_i = A("iota_i", [128, 16], mybir.dt.int32)
        self.lng = A("lng", [128, D], F32)
        self.lnb = A("lnb", [128, D], F32)
        self.epsc = A("epsc", [128, 1], F32)
        s.op("pool", lambda e: e.memset(self.epsc[:], EPS), writes=["epsc"])
        s.op("pool", lambda e: e.memset(self.ones_f[:], 1.0), writes=["ones_f"])
        s.op("pool", lambda e: e.affine_select(out=self.ident_f[:], in_=self.ones_f[:], pattern=[[-1, 128]],
                                               compare_op=ALU.is_equal, fill=0.0, base=0, channel_multiplier=1),
             reads=["ones_f"], writes=["ident_f"])
        s.op("pool", lambda e: e.tensor_copy(out=self.ident_b[:], in_=self.ident_f[:]), reads=["ident_f"], writes=["ident_b"])
        s.op("pool", lambda e: e.affine_select(out=self.triu[:], in_=self.ones_f[:], pattern=[[1, 128]],
                                               compare_op=ALU.is_ge, fill=0.0, base=0, channel_multiplier=-1),
             reads=["ones_f"], writes=["triu"])
        s.op("pool", lambda e: e.tensor_scalar(out=self.maskc[:], in0=self.triu[:], scalar1=QSCALE, scalar2=None, op0=ALU.mult),
             reads=["triu"], writes=["maskc"])
        s.op("pool", lambda e: e.iota(self.iota_i[:], pattern=[[1, 16]], base=1, channel_multiplier=0), writes=["iota_i"])
        for g in range(4):
            w = float(2 ** (g + 1))
            s.op("dve", lambda e, g=g, w=w: e.tensor_scalar(out=self.rdiv[:, g, :], in0=self.iota_i[:], scalar1=w, scalar2=None, op0=ALU.min),
                 reads=["iota_i"], writes=["rdiv%d" % g])
            s.op("dve", lambda e, g=g: e.reciprocal(out=self.rdiv[:, g, :], in_=self.rdiv[:, g, :]), reads=["rdiv%d" % g], writes=["rdiv%d" % g])
        s.dma("sp", "c0", self.rw[:], self.inp["router_w"].rearrange("(kc p) n -> p kc n", p=128), writes=["rw"])
        s.dma("sp", "c1", self.rb[:], self.inp["router_bias"][0:1, :].broadcast_to([128, 16]), writes=["rb"])

    def transpose_to_xT(self, src, srckeys, b, ti, xTf=None, xTfkey=None):
        s = self.s
        t = b * TPB + ti
        si = self.xTs_n % 2
        self.xTs_n += 1
        stg = self.xTs[si]
        for hf in range(2):
            bk = self.bank()
            pk = "ps%d" % bk
            pt = self.ps[bk]
            for c in range(4):
                kc = hf * 4 + c
                s.op("pe", lambda e, pt=pt, c=c, kc=kc: e.transpose(pt[:, c * 128:(c + 1) * 128], src[:, kc * 128:(kc + 1) * 128], self.ident_f[:]),
                     reads=list(srckeys) + ["ident_f"], writes=[pk])
            dst = stg[:, hf * 4:(hf + 1) * 4, :]
            srcv = pt[:].rearrange("p (c t) -> p c t", c=4)
            xk = "xTs%d_%d" % (si, hf)
            if xTf is not None:
                s.op("dve", lambda e, srcv=srcv, hf=hf: e.tensor_copy(out=xTf[:, hf * 4:(hf + 1) * 4, :], in_=srcv), reads=[pk], writes=[xTfkey + str(hf)])
                s.op("act", lambda e, dst=dst, srcv=srcv: e.copy(dst, srcv), reads=[pk], writes=[xk])
            elif hf == 0:
                s.op("dve", lambda e, dst=dst, srcv=srcv: e.tensor_copy(out=dst, in_=srcv), reads=[pk], writes=[xk])
            else:
                s.op("act", lambda e, dst=dst, srcv=srcv: e.copy(dst, srcv), reads=[pk], writes=[xk])
        s.dma("pool", self.chan("xTst", 3), self.xTd[t], stg[:], reads=["xTs%d_0" % si, "xTs%d_1" % si], writes=["xTd_%d" % t])

    def load_xT_tile(self, dst_ap, t, wkeys):
        self.s.dma("sp", self.chan("xTld", 4), dst_ap, self.xTd[t], reads=["xTd_%d" % t], writes=wkeys)

    def load_x(self):
        pass

    def run_pipe_range(self, lo, hi, stages):
        kmin = lo + min(o for o, _ in stages)
        kmax = hi - 1 + max(o for o, _ in stages)
        order = sorted(stages, key=lambda st_: -st_[0])
        for k in range(kmin, kmax + 1):
            for off, fn in order:
                tt_ = k - off
                if lo <= tt_ < hi:
                    fn(tt_)

    def run_pipe(self, n, stages):
        stages = [(st_ + ("tile", 0))[:4] if len(st_) == 2 else ((st_ + (0,))[:4]) for st_ in stages]
        kmin = min(st_[0] for st_ in stages)
        kmax = n - 1 + max(st_[0] for st_ in stages)
        order = sorted(stages, key=lambda st_: (-st_[3], -st_[0]))
        for k in range(kmin, kmax + 1):
            for off, fn, pool, _prio in order:
                t = k - off
                if 0 <= t < n:
                    self.cur_pool = pool
                    fn(t)
        self.cur_pool = None

    def ep_alloc(self, st, router=False, nt=2):
        ep = {"nt": nt}
        ep["xres"] = [self.sb(st, "xres", [128, D], F32) for _ in range(2)]
        ep["t"] = [self.sb(st, "tt", [128, D], F32) for _ in range(nt)]
        ep["st6"] = [self.sb(st, "st6", [128, 2, 6], F32) for _ in range(nt)]
        ep["mv"] = [self.sb(st, "mv", [128, 2], F32) for _ in range(nt)]
        ep["sm"] = [self.sb(st, "esm", [128, 4], F32) for _ in range(nt)]
        if router:
            ep["xTf"] = [self.sb(st, "xTf", [128, 8, 128], F32) for _ in range(1)]
            ep["r"] = [[self.sb(st, "r%d" % j, [128, 16], F32) for j in range(8)] for _ in range(2)]
        return ep

    def ep1(self, ep, t, halves, hkeys, src_dram, sname):
        s = self.s
        i2 = t % 2
        i = t % ep["nt"]
        xres, tt, st6, mv, sm = ep["xres"][i2], ep["t"][i], ep["st6"][i], ep["mv"][i], ep["sm"][i]
        KX = "ep_xres%d" % i2
        K = lambda n: "ep_%s%d" % (n, i)
        s.dma("sp", self.chan("xl", 3), xres[:], src_dram[t * 128:(t + 1) * 128, :], reads=["dram_%s_%d" % (sname, t)], writes=[KX])
        for hf in range(2):
            sl = slice(hf * 512, (hf + 1) * 512)
            s.op("dve", lambda e, hf=hf, sl=sl: e.scalar_tensor_tensor(out=tt[:, sl], in0=xres[:, sl], scalar=ALPHA, in1=halves[hf], op0=ALU.mult, op1=ALU.add),
                 reads=[KX, hkeys[hf]], writes=[K("t%d" % hf)])
            s.op("dve", lambda e, hf=hf, sl=sl: e.bn_stats(out=st6[:, hf, :], in_=tt[:, sl]), reads=[K("t%d" % hf)], writes=[K("st%d" % hf)])
        s.op("dve", lambda e: e.bn_aggr(out=mv[:], in_=st6[:].rearrange("p a b -> p (a b)")), reads=[K("st0"), K("st1")], writes=[K("mv")])
        s.op("act", lambda e: e.activation(out=sm[:, 0:1], in_=mv[:, 1:2], func=AF.Ln, bias=self.epsc[:], scale=1.0), reads=[K("mv"), "epsc"], writes=[K("sm")])
        s.op("act", lambda e: e.activation(out=sm[:, 1:2], in_=sm[:, 0:1], func=AF.Exp, scale=-0.5), reads=[K("sm")], writes=[K("sm")])

    def ep1b(self, ep, t):
        s = self.s
        i = t % ep["nt"]
        tt, sm, mv = ep["t"][i], ep["sm"][i], ep["mv"][i]
        K = lambda n: "ep_%s%d" % (n, i)
        tk = [K("t0"), K("t1")]
        s.op("dve", lambda e: e.scalar_tensor_tensor(out=sm[:, 2:3], in0=mv[:, 0:1], scalar=-1.0, in1=sm[:, 1:2], op0=ALU.mult, op1=ALU.mult), reads=[K("mv"), K("sm")], writes=[K("sm")])
        s.op("act", lambda e: e.activation(out=tt[:], in_=tt[:], func=AF.Identity, bias=sm[:, 2:3], scale=sm[:, 1:2]), reads=tk + [K("sm")], writes=tk)

    def ep2a(self, ep, t, dst_dram, dname):
        s = self.s
        i = t % ep["nt"]
        tt, sm = ep["t"][i], ep["sm"][i]
        K = lambda n: "ep_%s%d" % (n, i)
        tk = [K("t0"), K("t1")]
        s.op("dve", lambda e: e.tensor_tensor(out=tt[:], in0=tt[:], in1=self.lng[:], op=ALU.mult), reads=tk + ["lng"], writes=tk)
        s.op("dve", lambda e: e.tensor_tensor(out=tt[:], in0=tt[:], in1=self.lnb[:], op=ALU.add), reads=tk + ["lnb"], writes=tk)
        s.dma("pool", self.chan("xs", 3), dst_dram[t * 128:(t + 1) * 128, :], tt[:], reads=tk, writes=["dram_%s_%d" % (dname, t)])

    def ep2b(self, ep, t, router=False):
        b, ti = divmod(t, TPB)
        i = t % ep["nt"]
        tt = ep["t"][i]
        K = lambda n: "ep_%s%d" % (n, i)
        tk = [K("t0"), K("t1")]
        if router:
            xTf = ep["xTf"][0]
            self.transpose_to_xT(tt, tk, b, ti, xTf=xTf, xTfkey="ep_xTf")
            self.route(ep, t % 2, t, xTf, ["ep_xTf0", "ep_xTf1"])
        else:
            self.transpose_to_xT(tt, tk, b, ti)

    def route(self, ep, i, t, xTf, xkeys):
        s = self.s
        bk = self.bank()
        pk = "ps%d" % bk
        lg = self.ps[bk][:, 0:16]
        for kc in range(8):
            s.op("pe", lambda e, kc=kc: e.matmul(lg, xTf[:, kc, :], self.rw[:, kc, :], start=(kc == 0), stop=(kc == 7)), reads=xkeys + ["rw"], writes=[pk])
        sc, sel, eq, m, t4, ge, msk, gsel = ep["r"][i]
        R = lambda n: "rt_%s%d" % (n, i)
        v4 = lambda a: a[:].rearrange("p (g k) -> p g k", g=4)
        bc = lambda a, c0: a[:, c0:c0 + 4].unsqueeze(2).broadcast_to([128, 4, 4])
        s.op("act", lambda e: e.activation(out=sc[:], in_=lg, func=AF.Exp, scale=-1.0), reads=[pk], writes=[R("sc")])
        s.op("dve", lambda e: e.tensor_scalar(out=sc[:], in0=sc[:], scalar1=1.0, scalar2=None, op0=ALU.add), reads=[R("sc")], writes=[R("sc")])
        s.op("dve", lambda e: e.reciprocal(out=sc[:], in_=sc[:]), reads=[R("sc")], writes=[R("sc")])
        s.op("dve", lambda e: e.tensor_tensor(out=sel[:], in0=sc[:], in1=self.rb[:], op=ALU.add), reads=[R("sc"), "rb"], writes=[R("sel")])
        s.op("dve", lambda e: e.tensor_reduce(out=m[:, 0:4], in_=v4(sel), axis=AX.X, op=ALU.max), reads=[R("sel")], writes=[R("m")])
        s.op("dve", lambda e: e.tensor_tensor(out=v4(eq), in0=v4(sel), in1=bc(m, 0), op=ALU.is_equal), reads=[R("sel"), R("m")], writes=[R("eq")])
        s.op("dve", lambda e: e.scalar_tensor_tensor(out=eq[:], in0=eq[:], scalar=-1e9, in1=sel[:], op0=ALU.mult, op1=ALU.add), reads=[R("eq"), R("sel")], writes=[R("eq")])
        s.op("dve", lambda e: e.tensor_reduce(out=m[:, 4:8], in_=v4(eq), axis=AX.X, op=ALU.max), reads=[R("eq"), R("m")], writes=[R("m")])
        s.op("dve", lambda e: e.tensor_tensor(out=m[:, 8:12], in0=m[:, 0:4], in1=m[:, 4:8], op=ALU.add), reads=[R("m")], writes=[R("m")])
        s.op("dve", lambda e: e.tensor_reduce(out=m[:, 12:13], in_=m[:, 8:12], axis=AX.X, op=ALU.max), reads=[R("m")], writes=[R("m")])
        s.op("dve", lambda e: e.tensor_scalar(out=t4[:, 0:4], in0=m[:, 8:12], scalar1=m[:, 12:13], scalar2=None, op0=ALU.is_ge), reads=[R("m")], writes=[R("t4")])
        s.op("dve", lambda e: e.tensor_tensor(out=v4(ge), in0=v4(sel), in1=bc(m, 4), op=ALU.is_ge), reads=[R("sel"), R("m")], writes=[R("ge")])
        s.op("dve", lambda e: e.tensor_tensor(out=v4(msk), in0=v4(ge), in1=bc(t4, 0), op=ALU.mult), reads=[R("ge"), R("t4")], writes=[R("msk")])
        s.op("dve", lambda e: e.tensor_tensor(out=gsel[:], in0=sc[:], in1=msk[:], op=ALU.mult), reads=[R("sc"), R("msk")], writes=[R("gsel")])
        s.op("dve", lambda e: e.tensor_reduce(out=m[:, 13:14], in_=gsel[:], axis=AX.X, op=ALU.add), reads=[R("gsel"), R("m")], writes=[R("m")])
        s.op("dve", lambda e: e.reciprocal(out=m[:, 14:15], in_=m[:, 13:14]), reads=[R("m")], writes=[R("m")])
        s.op("dve", lambda e: e.tensor_scalar(out=self.wgt[:, t, :], in0=gsel[:], scalar1=m[:, 14:15], scalar2=None, op0=ALU.mult), reads=[R("gsel"), R("m")], writes=["wgt%d" % t])

    def mixer(self, l, src, sname, dst, dname):
        import contextlib
        s, nc = self.s, self.nc
        BLK, TB = 256, 2
        NQ, NV, NO, NM = 4, 3, 4, 4
        with contextlib.ExitStack() as st:
            SB = lambda n, shp, dt: self.sb(st, n, shp, dt)
            w_in = SB("w_in", [128, 8, N_IN], BF16)
            w_mo = SB("w_mo", [128, 8, D], BF16)
            pw = SB("pw", [128, 4, 128], BF16)
            cvw = SB("cvw", [128, 4, 8], F32)
            psc = SB("psc", [128, 4], F32)
            hng = SB("hng", [128, M], F32)
            bif = SB("bif", [128, 8], F32)
            wsrc = self.inp["w_in"][l].rearrange("(kc p) n -> p kc n", p=128)
            groups = [(0, 512), (512, 1024), (2048, N_IN), (1024, 1536), (1536, 2048)]
            gkey = {0: 0, 512: 1, 2048: 4, 1024: 2, 1536: 3}
            for (a, b_) in groups:
                s.dma("pool", self.chan("wl", 4), w_in[:, :, a:b_], wsrc[:, :, a:b_], writes=["w_in%d" % gkey[a]])
            msrc = self.inp["w_mix_out"][l].rearrange("(kc p) n -> p kc n", p=128)
            for hf in range(2):
                s.dma("pool", self.chan("wl", 4), w_mo[:, :, hf * 512:(hf + 1) * 512], msrc[:, :, hf * 512:(hf + 1) * 512], writes=["w_mo%d" % hf])
            s.dma("pool", self.chan("wl", 4), pw[:], self.inp["pool_w"][l].rearrange("g c d -> c g d"), writes=["pw"])
            for j4 in range(4):
                self.dma_nc("sp", "c0", cvw[:, j4, :], self.inp["conv_qk"][l, j4].rearrange("(k c) -> c k", c=128), writes=["cvw"])
            self.dma_nc("sp", "c1", psc[:], self.inp["pool_scale"][l].rearrange("(g c) -> c g", c=128), writes=["psc"])
            s.dma("sp", "c0", hng[:], self.inp["head_norm_g"][l:l + 1, :].broadcast_to([128, M]), writes=["hng"])
            s.dma("sp", "c1", bif[:, 0:4], self.inp["b_i"][l:l + 1, :].broadcast_to([128, 4]), writes=["bif"])
            s.dma("sp", "c0", bif[:, 4:8], self.inp["b_f"][l:l + 1, :].broadcast_to([128, 4]), writes=["bif"])
            self.load_ln("ln_mix_g", "ln_mix_b", l)

            W = BLK + 16
            xTb = [SB("xTb", [128, 8, BLK], BF16) for _ in range(2)]
            zq = [SB("zq", [128, BLK + 3], F32) for _ in range(3)]
            zh = SB("zh", [128, 8, 3], F32)
            PE_CONV = (0, 3, 5, 6)
            acc = [SB("acc", [128, BLK], F32) for _ in range(2)]
            dg = SB("dg", [128, 32, 128], F32)
            for j4 in range(4):
                for c8 in PE_CONV:
                    s.op("dve", lambda e, j4=j4, c8=c8: e.tensor_scalar(out=dg[:, j4 * 8 + c8, :], in0=self.ident_f[:], scalar1=cvw[:, j4, c8:c8 + 1], scalar2=None, op0=ALU.mult),
                         reads=["ident_f", "cvw"], writes=["dg"])
            qkT = [SB("qkT", [128, 8, BLK], BF16) for _ in range(NQ)]
            ktok = [SB("ktok", [128, TB, 512], BF16) for _ in range(NV)]
            vext = [SB("vext", [128, TB, 4, 130], BF16) for _ in range(NV)]
            og = [SB("og", [128, TB, 512], BF16) for _ in range(NO)]
            gts = [SB("gt", [128, 32], F32) for _ in range(NO * TB)]
            pT = [SB("pT", [128, 4, 128], BF16) for _ in range(2)]
            hraw = [SB("hraw", [128, 512], F32) for _ in range(2)]
            hst = [SB("hst", [128, 4, 6], F32) for _ in range(2)]
            hmv = [SB("hmv", [128, 4, 2], F32) for _ in range(2)]
            hrs = [SB("hrs", [128, 8], F32) for _ in range(2)]
            hb = [SB("hb", [128, 512], BF16) for _ in range(2)]
            Cn = SB("Cn", [128, 4, 130], F32)
            Cnb = SB("Cnb", [128, 4, 130], BF16)
            ub = [SB("ub", [128, W], F32) for _ in range(2)]
            uh = SB("uh", [128, 4, 16], F32)
            sAB = [[SB("sA", [128, W], F32), SB("sB", [128, W], F32)] for _ in range(2)]
            pooledT = [SB("pooledT", [128, BLK], BF16) for _ in range(8)]
            mixedT = [SB("mixedT", [128, 8, BLK], BF16) for _ in range(NM)]
            ep = self.ep_alloc(st, nt=3)
            if l == 0 and sname == "x":
                for t_ in range(NT):
                    b_, ti_ = divmod(t_, TPB)
                    buf = ep["xres"][t_ % 2] if t_ % 3 < 2 else ep["t"][0]
                    k_ = "ep_xres%d" % (t_ % 2) if t_ % 3 < 2 else None
                    buf, k_ = (ep["xres"][t_ % 2], "ep_xres%d" % (t_ % 2))
                    s.dma("sp", self.chan("xl", 3), buf[:], self.inp["x"][t_ * 128:(t_ + 1) * 128, :], writes=[k_])
                    self.transpose_to_xT(buf, [k_], b_, ti_)
            for r in range(NV):
                s.op("dve", lambda e, r=r: e.memset(vext[r][:], 1.0), writes=["vext%d_%d" % (r, i) for i in range(TB)])
            cnt = {"z": 0, "u": 0}
            pend = {"pool": []}

            def S0a(t):
                if t % TB:
                    return
                bg = t // TB
                rq = bg % NQ
                rx = bg % 2
                b, ti0 = divmod(t, TPB)
                if ti0 == 0:
                    s.op("dve", lambda e: e.memset(zh[:], 0.0), writes=["zh%d" % c for c in range(8)])
                xk = ["xTb%d_%d" % (rx, i) for i in range(TB)]
                prev_conv = None
                for c in range(8):
                    zi = cnt["z"] % 3
                    cnt["z"] += 1
                    zq_ = zq[zi]
                    ZK = "zq%d" % zi
                    bk = self.bank(); pk = "ps%d" % bk; pz = self.ps[bk]
                    self.mm_acc(pz[:, 0:BLK], pk, lambda kc, c=c: w_in[:, kc, c * 128:(c + 1) * 128], lambda kc, rx=rx: xTb[rx][:, kc, :], 8, xk + ["w_in%d" % (c // 4)])
                    s.op("act", lambda e, pz=pz, zq_=zq_: e.copy(zq_[:, 3:BLK + 3], pz[:, 0:BLK]), reads=[pk], writes=[ZK])
                    s.op("act", lambda e, c=c, zq_=zq_: e.copy(zq_[:, 0:3], zh[:, c, :]), reads=["zh%d" % c], writes=[ZK])
                    s.op("act", lambda e, c=c, zq_=zq_: e.copy(zh[:, c, :], zq_[:, BLK:BLK + 3]), reads=[ZK], writes=["zh%d" % c])
                    if prev_conv is not None:
                        prev_conv()

                    def conv(c=c, zq_=zq_, ZK=ZK, rq=rq):
                        if c not in PE_CONV:
                            ai = cnt["z"] % 2
                            acc_ = acc[ai]
                            AK = "acc%d" % ai
                            s.op("act", lambda e: e.activation(out=acc_[:], in_=zq_[:, 0:BLK], func=AF.Copy, scale=cvw[:, 0, c:c + 1]), reads=[ZK, "cvw"], writes=[AK])
                            for j2 in range(1, 4):
                                s.op("dve", lambda e, j2=j2: e.scalar_tensor_tensor(out=acc_[:], in0=zq_[:, j2:j2 + BLK], scalar=cvw[:, j2, c:c + 1], in1=acc_[:],
                                                                                  op0=ALU.mult, op1=ALU.add), reads=[ZK, "cvw", AK], writes=[AK])
                            s.op("act", lambda e: e.activation(out=qkT[rq][:, c, :], in_=acc_[:], func=AF.Silu), reads=[AK], writes=["qkT%d_%d" % (rq, c)])
                            return
                        bk2 = self.bank(); pk2 = "ps%d" % bk2; pc_ = self.ps[bk2]
                        for j2 in range(4):
                            s.op("pe", lambda e, pc_=pc_, j2=j2, c=c, zq_=zq_: e.matmul(pc_[:, 0:BLK], dg[:, j2 * 8 + c, :], zq_[:, j2:j2 + BLK], start=(j2 == 0), stop=(j2 == 3)),
                                 reads=[ZK, "dg"], writes=[pk2])
                        s.op("act", lambda e, pc_=pc_, c=c, rq=rq: e.activation(out=qkT[rq][:, c, :], in_=pc_[:, 0:BLK], func=AF.Silu), reads=[pk2], writes=["qkT%d_%d" % (rq, c)])
                    prev_conv = conv
                prev_conv()

            def S0x(t):
                if t % TB:
                    return
                rx = (t // TB) % 2
                for i in range(TB):
                    self.load_xT_tile(xTb[rx][:, :, i * 128:(i + 1) * 128], t + i, ["xTb%d_%d" % (rx, i)])

            def S0b(t):
                if t % TB != 1:
                    return
                t0 = t - 1
                bg = t0 // TB
                rq, rv, ro, rm, rx = bg % NQ, bg % NV, bg % NO, bg % NM, bg % 2
                b, ti0 = divmod(t0, TPB)
                if ti0 == 0:
                    s.op("dve", lambda e: e.memset(uh[:], 0.0), writes=["uh%d" % g for g in range(4)])
                xk = ["xTb%d_%d" % (rx, i) for i in range(TB)]
                for g in range(4):
                    ui = cnt["u"] % 2
                    cnt["u"] += 1
                    pi = (bg % 2) * 4 + g
                    ub_, (sA, sB_), pl_ = ub[ui], sAB[ui], pooledT[pi]
                    UK, SAK, SBK, PLK = "ub%d" % ui, "sA%d" % ui, "sB%d" % ui, "pooledT%d" % pi
                    bk = self.bank(); pk = "ps%d" % bk; pz = self.ps[bk]
                    col0 = 2056 + g * 128
                    self.mm_acc(pz[:, 0:BLK], pk, lambda kc, col0=col0: w_in[:, kc, col0:col0 + 128], lambda kc, rx=rx: xTb[rx][:, kc, :], 8, xk + ["w_in4"])
                    s.op("act", lambda e, pz=pz, ub_=ub_: e.copy(ub_[:, 16:W], pz[:, 0:BLK]), reads=[pk], writes=[UK])
                    s.op("act", lambda e, g=g, ub_=ub_: e.copy(ub_[:, 0:16], uh[:, g, :]), reads=["uh%d" % g], writes=[UK])
                    s.op("act", lambda e, g=g, ub_=ub_: e.copy(uh[:, g, :], ub_[:, BLK:W]), reads=[UK], writes=["uh%d" % g])
                    cur, curk, lo = ub_, UK, 0
                    for step in range(g + 1):
                        sh = 2 ** step
                        nxt, nxtk = (sA, SAK) if step % 2 == 0 else (sB_, SBK)
                        lo2 = lo + sh
                        s.op("dve", lambda e, cur=cur, nxt=nxt, lo2=lo2, sh=sh: e.tensor_tensor(out=nxt[:, lo2:W], in0=cur[:, lo2:W], in1=cur[:, lo2 - sh:W - sh], op=ALU.add),
                             reads=[curk], writes=[nxtk])
                        cur, curk, lo = nxt, nxtk, lo2
                    w = float(2 ** (g + 1))
                    s.op("dve", lambda e, cur=cur, w=w, ub_=ub_, pl_=pl_: e.scalar_tensor_tensor(out=pl_[:], in0=cur[:, 16:W], scalar=1.0 / w, in1=ub_[:, 16:W],
                                                                                               op0=ALU.mult, op1=ALU.subtract), reads=[curk, UK], writes=[PLK])
                    if ti0 == 0:
                        oth, othk = (sB_, SBK) if cur is sA else (sA, SAK)
                        s.op("dve", lambda e, cur=cur, oth=oth, g=g: e.tensor_tensor(out=oth[:, 0:16], in0=cur[:, 16:32], in1=self.rdiv[:, g, :], op=ALU.mult),
                             reads=[curk, "rdiv%d" % g], writes=[othk])
                        s.op("dve", lambda e, oth=oth, ub_=ub_, pl_=pl_: e.tensor_tensor(out=pl_[:, 0:16], in0=oth[:, 0:16], in1=ub_[:, 16:32], op=ALU.subtract),
                             reads=[othk, UK], writes=[PLK])
                    pend["pool"].append((g, pl_, PLK, rm))
                lhs_t = lambda i: (lambda kc, i=i, rx=rx: xTb[rx][:, kc, i * 128:(i + 1) * 128])
                for i in range(TB):
                    bk = self.bank(); pk = "ps%d" % bk; pz = self.ps[bk]
                    self.mm_acc(pz[:], pk, lhs_t(i), lambda kc: w_in[:, kc, 1024:1536], 8, ["xTb%d_%d" % (rx, i), "w_in2"])
                    s.op("act", lambda e, pz=pz, i=i, rv=rv: e.copy(vext[rv][:, i, :, 0:128], pz[:].rearrange("p (h e) -> p h e", h=4)), reads=[pk], writes=["vext%d_%d" % (rv, i)])
                for i in range(TB):
                    bk = self.bank(); pk = "ps%d" % bk; pz = self.ps[bk]
                    self.mm_acc(pz[:], pk, lhs_t(i), lambda kc: w_in[:, kc, 1536:2048], 8, ["xTb%d_%d" % (rx, i), "w_in3"])
                    s.op("act", lambda e, pz=pz, i=i, ro=ro: e.activation(out=og[ro][:, i, :], in_=pz[:], func=AF.Sigmoid), reads=[pk], writes=["og%d_%d" % (ro, i)])
                for i in range(TB):
                    gi = ro * TB + i
                    gt = gts[gi]
                    G = "G%d" % gi
                    bk = self.bank(); pk = "ps%d" % bk; pz = self.ps[bk]
                    self.mm_acc(pz[:, 0:8], pk, lhs_t(i), lambda kc: w_in[:, kc, 2048:2056], 8, ["xTb%d_%d" % (rx, i), "w_in4"])
                    s.op("dve", lambda e, pz=pz, gt=gt: e.tensor_tensor(out=gt[:, 0:8], in0=pz[:, 0:8], in1=bif[:], op=ALU.add), reads=[pk, "bif"], writes=[G])
                    s.op("act", lambda e, gt=gt: e.activation(out=gt[:, 8:12], in_=gt[:, 4:8], func=AF.Exp, scale=-1.0), reads=[G], writes=[G])
                    s.op("act", lambda e, gt=gt: e.activation(out=gt[:, 8:12], in_=gt[:, 8:12], func=AF.Ln, bias=1.0), reads=[G], writes=[G])

            def S0c(t):
                if t % TB:
                    return
                bg = t // TB
                rq, rv, ro, rm = bg % NQ, bg % NV, bg % NO, bg % NM
                for (g, pl_, PLK, rm_) in pend["pool"]:
                    bk2 = self.bank(); pk2 = "ps%d" % bk2; pz2 = self.ps[bk2]
                    s.op("pe", lambda e, pz2=pz2, g=g, pl_=pl_: e.matmul(pz2[:, 0:BLK], pw[:, g, :], pl_[:], start=True, stop=True), reads=["pw", PLK], writes=[pk2])
                    s.op("act", lambda e, pz2=pz2, g=g, rm_=rm_: e.activation(out=mixedT[rm_][:, 4 + g, :], in_=pz2[:, 0:BLK], func=AF.Copy, scale=psc[:, g:g + 1]),
                         reads=[pk2, "psc"], writes=["mxp%d_%d" % (rm_, g)])
                pend["pool"] = []
                for i in range(TB):
                    gi = ro * TB + i
                    gt = gts[gi]
                    G = "G%d" % gi
                    bk = self.bank(); pkc = "ps%d" % bk; pc = self.ps[bk]
                    s.op("pe", lambda e, pc=pc, gt=gt: e.matmul(pc[:, 0:4], self.triu[:], gt[:, 8:12], start=True, stop=True), reads=[G, "triu"], writes=[pkc])
                    s.op("pe", lambda e, pc=pc, gt=gt: e.matmul(pc[:, 4:8], self.ones_f[:], gt[:, 8:12], start=True, stop=True), reads=[G, "ones_f"], writes=[pkc])
                    s.op("dve", lambda e, pc=pc, gt=gt: e.tensor_tensor(out=gt[:, 12:16], in0=gt[:, 0:4], in1=pc[:, 0:4], op=ALU.add), reads=[G, pkc], writes=[G])
                    s.op("act", lambda e, gt=gt: e.activation(out=gt[:, 12:16], in_=gt[:, 12:16], func=AF.Exp), reads=[G], writes=[G])
                    s.op("act", lambda e, pc=pc, gt=gt: e.activation(out=gt[:, 16:24], in_=pc[:, 0:8], func=AF.Exp, scale=-1.0), reads=[G, pkc], writes=[G])
                    bk = self.bank(); pk = "ps%d" % bk; pzb = self.psb[bk]
                    for h in range(4):
                        s.op("pe", lambda e, pzb=pzb, h=h, i=i, rq=rq: e.transpose(pzb[:, h * 128:(h + 1) * 128], qkT[rq][:, 4 + h, i * 128:(i + 1) * 128], self.ident_b[:]),
                             reads=["qkT%d_%d" % (rq, 4 + h), "ident_b"], writes=[pk])
                    for h in range(4):
                        s.op("act", lambda e, pzb=pzb, h=h, i=i, rv=rv, gt=gt: e.activation(out=ktok[rv][:, i, h * 128:(h + 1) * 128], in_=pzb[:, h * 128:(h + 1) * 128], func=AF.Copy, scale=gt[:, 12 + h:13 + h]),
                             reads=[pk, G], writes=["ktok%d_%d" % (rv, i)])


            def S1a(t):
                bg = t // TB
                rq, ro = bg % NQ, bg % NO
                i = t % TB
                gi = ro * TB + i
                gt = gts[gi]
                G = "G%d" % gi
                p2 = t % 2
                cols = slice(i * 128, (i + 1) * 128)
                bk = self.bank(); pkS = "ps%d" % bk; pS = self.ps[bk]
                for h in range(4):
                    s.op("pe", lambda e, pS=pS, h=h: e.matmul(pS[:, h * 128:(h + 1) * 128], qkT[rq][:, 4 + h, cols], qkT[rq][:, h, cols], start=True, stop=True),
                         reads=["qkT%d_%d" % (rq, h), "qkT%d_%d" % (rq, 4 + h)], writes=[pkS])
                for h in range(4):
                    s.op("dve", lambda e, pS=pS, h=h: e.scalar_tensor_tensor(out=pT[p2][:, h, :], in0=pS[:, h * 128:(h + 1) * 128], scalar=gt[:, 12 + h:13 + h], in1=self.maskc[:],
                                                                           op0=ALU.mult, op1=ALU.mult), reads=[pkS, G, "maskc"], writes=["pT%d_%d" % (p2, h)])

            def S1b(t):
                bg = t // TB
                rq, rv, ro = bg % NQ, bg % NV, bg % NO
                i = t % TB
                b, ti = divmod(t, TPB)
                gi = ro * TB + i
                gt = gts[gi]
                G = "G%d" % gi
                p2 = t % 2
                cols = slice(i * 128, (i + 1) * 128)
                if ti == 0:
                    s.op("dve", lambda e: e.memset(Cn[:], 0.0), writes=["Cn"])
                    s.op("dve", lambda e: e.memset(Cnb[:], 0.0), writes=["Cnb"])
                pO, pkO, pSt, pkSt = [], [], [], []
                for bb in range(2):
                    bk = self.bank(); pkO.append("ps%d" % bk); pO.append(self.ps[bk][:, 0:260].rearrange("p (h e) -> p h e", h=2))
                for bb in range(2):
                    bk = self.bank(); pkSt.append("ps%d" % bk); pSt.append(self.ps[bk][:, 0:260].rearrange("p (h e) -> p h e", h=2))
                for h in range(4):
                    o_ap = pO[h // 2][:, h % 2, 0:129]
                    s.op("pe", lambda e, o_ap=o_ap, h=h: e.matmul(o_ap, pT[p2][:, h, :], vext[rv][:, i, h, 0:129], start=True, stop=False),
                         reads=["pT%d_%d" % (p2, h), "vext%d_%d" % (rv, i)], writes=[pkO[h // 2]])
                    s.op("pe", lambda e, o_ap=o_ap, h=h: e.matmul(o_ap, qkT[rq][:, h, cols], Cnb[:, h, 0:129], start=False, stop=True),
                         reads=["qkT%d_%d" % (rq, h), "Cnb"], writes=[pkO[h // 2]])
                for h in range(4):
                    st_ap = pSt[h // 2][:, h % 2, 0:129]
                    s.op("pe", lambda e, st_ap=st_ap, h=h: e.matmul(st_ap, ktok[rv][:, i, h * 128:(h + 1) * 128], vext[rv][:, i, h, 0:129], start=True, stop=True),
                         reads=["ktok%d_%d" % (rv, i), "vext%d_%d" % (rv, i)], writes=[pkSt[h // 2]])
                for bb in range(2):
                    s.op("dve", lambda e, bb=bb, po=pO[bb]: e.tensor_tensor(out=gt[:, 24 + 2 * bb:26 + 2 * bb], in0=po[:, :, 128], in1=gt[:, 16 + 2 * bb:18 + 2 * bb], op=ALU.mult),
                         reads=[pkO[bb], G], writes=[G])
                s.op("dve", lambda e: e.tensor_scalar(out=gt[:, 28:32], in0=gt[:, 24:28], scalar1=1.0, scalar2=None, op0=ALU.max), reads=[G], writes=[G])
                s.op("dve", lambda e: e.scalar_tensor_tensor(out=gt[:, 24:28], in0=gt[:, 24:28], scalar=-1.0, in1=gt[:, 28:32], op0=ALU.mult, op1=ALU.max), reads=[G], writes=[G])
                s.op("dve", lambda e: e.reciprocal(out=gt[:, 24:28], in_=gt[:, 24:28]), reads=[G], writes=[G])
                s.op("dve", lambda e: e.tensor_tensor(out=gt[:, 28:32], in0=gt[:, 24:28], in1=gt[:, 16:20], op=ALU.mult), reads=[G], writes=[G])
                for h in range(4):
                    s.op("act", lambda e, h=h, po=pO[h // 2]: e.activation(out=hraw[p2][:, h * 128:(h + 1) * 128], in_=po[:, h % 2, 0:128], func=AF.Copy, scale=gt[:, 28 + h:29 + h]),
                         reads=[pkO[h // 2], G], writes=["hraw%d_%d" % (h, p2)])
                for bb in range(2):
                    s.op("dve", lambda e, bb=bb, pst=pSt[bb]: e.tensor_tensor(out=Cn[:, 2 * bb:2 * bb + 2, 0:129], in0=pst[:, :, 0:129], in1=Cn[:, 2 * bb:2 * bb + 2, 0:129], op=ALU.add),
                         reads=[pkSt[bb], "Cn"], writes=["Cn"])
                s.op("dve", lambda e: e.tensor_tensor(out=Cn[:, :, 0:129], in0=Cn[:, :, 0:129], in1=gt[:, 20:24].unsqueeze(2).broadcast_to([128, 4, 129]), op=ALU.mult),
                     reads=["Cn", G], writes=["Cn"])
                s.op("dve", lambda e: e.tensor_scalar(out=Cnb[:].rearrange("p h e -> p (h e)"), in0=Cn[:].rearrange("p h e -> p (h e)"), scalar1=QSCALE, scalar2=None, op0=ALU.mult), reads=["Cn"], writes=["Cnb"])

            def S2(t):
                bg = t // TB
                ro, rm = bg % NO, bg % NM
                i = t % TB
                p2 = t % 2
                cols = slice(i * 128, (i + 1) * 128)
                hr_, hs_, hm_, hq_, hb_ = hraw[p2], hst[p2], hmv[p2], hrs[p2], hb[p2]
                HK = lambda n: "h%s%d" % (n, p2)
                for h in range(4):
                    s.op("dve", lambda e, h=h: e.bn_stats(out=hs_[:, h, :], in_=hr_[:, h * 128:(h + 1) * 128]), reads=[HK("raw%d_" % h)], writes=[HK("st%d_" % h)])
                    s.op("dve", lambda e, h=h: e.bn_aggr(out=hm_[:, h, :], in_=hs_[:, h, :]), reads=[HK("st%d_" % h)], writes=[HK("mv%d_" % h)])
                hmk = [HK("mv%d_" % h) for h in range(4)]
                s.op("act", lambda e: e.activation(out=hq_[:, 0:4], in_=hm_[:, :, 1], func=AF.Ln, bias=self.epsc[:], scale=1.0), reads=hmk + ["epsc"], writes=[HK("rs")])
                s.op("act", lambda e: e.activation(out=hq_[:, 4:8], in_=hq_[:, 0:4], func=AF.Exp, scale=-0.5), reads=[HK("rs")], writes=[HK("rs")])
                for h in range(4):
                    s.op("dve", lambda e, h=h: e.tensor_scalar(out=hr_[:, h * 128:(h + 1) * 128], in0=hr_[:, h * 128:(h + 1) * 128], scalar1=hm_[:, h, 0:1], scalar2=hq_[:, 4 + h:5 + h],
                                                             op0=ALU.subtract, op1=ALU.mult), reads=[HK("raw%d_" % h), HK("mv%d_" % h), HK("rs")], writes=[HK("raw%d_" % h)])
                hk = [HK("raw%d_" % h) for h in range(4)]
                s.op("dve", lambda e: e.tensor_tensor(out=hr_[:], in0=hr_[:], in1=hng[:], op=ALU.mult), reads=hk + ["hng"], writes=hk)
                s.op("dve", lambda e: e.tensor_tensor(out=hb_[:], in0=hr_[:], in1=og[ro][:, i, :], op=ALU.mult), reads=hk + ["og%d_%d" % (ro, i)], writes=[HK("b")])

            def S2b(t):
                bg = t // TB
                rm = bg % NM
                i = t % TB
                p2 = t % 2
                cols = slice(i * 128, (i + 1) * 128)
                hb_ = hb[p2]
                HK = lambda n: "h%s%d" % (n, p2)
                bk = self.bank(); pk = "ps%d" % bk; pzb = self.psb[bk]
                for h in range(4):
                    s.op("pe", lambda e, pzb=pzb, h=h: e.transpose(pzb[:, h * 128:(h + 1) * 128], hb_[:, h * 128:(h + 1) * 128], self.ident_b[:]), reads=[HK("b"), "ident_b"], writes=[pk])
                s.op("act", lambda e, pzb=pzb: e.copy(mixedT[rm][:, 0:4, cols], pzb[:, 0:512].rearrange("p (h t) -> p h t", h=4)), reads=[pk], writes=["mxh%d_%d" % (rm, i)])

            def S3(t):
                bg = t // TB
                rm = bg % NM
                i = t % TB
                cols = slice(i * 128, (i + 1) * 128)
                halves, hkeys = [], []
                for hf in range(2):
                    bk = self.bank(); pk = "ps%d" % bk; pz = self.ps[bk]
                    self.mm_acc(pz[:], pk, lambda kc: mixedT[rm][:, kc, cols], lambda kc, hf=hf: w_mo[:, kc, hf * 512:(hf + 1) * 512], 8,
                                ["mxh%d_%d" % (rm, i)] + ["mxp%d_%d" % (rm, g) for g in range(4)] + ["w_mo%d" % hf])
                    halves.append(pz[:]); hkeys.append(pk)
                self.ep1(ep, t, halves, hkeys, src, sname)

            def S3b(t):
                self.ep1b(ep, t)

            def S4a(t):
                self.ep2a(ep, t, dst, dname)

            def S4b(t):
                self.ep2b(ep, t)

            self.run_pipe(NT, [(-7, S0x, "blk"), (-5, S0a, "blk"), (-5, S0b, "blk"), (-3, S0c, "blk"), (0, S1a, "tile", 1), (1, S1b, "tile", 2), (2, S2), (3, S2b), (4, S3), (5, S3b), (6, S4a), (7, S4b)])
            self.barrier()

    def xattn(self, l, src, sname, dst, dname):
        import contextlib
        s, nc = self.s, self.nc
        BLK, TB = 256, 2
        with contextlib.ExitStack() as st:
            SB = lambda n, shp, dt: self.sb(st, n, shp, dt)
            wq = SB("wq", [128, 8, D], BF16)
            wo = SB("wo", [128, 8, D], BF16)
            wkv = SB("wkv", [128, 8, 2 * D], BF16)
            for nm, wt, key, ncol in (("w_xkv", wkv, "wkv", 2048), ("w_xq", wq, "wq", 1024), ("w_xo", wo, "wo", 1024)):
                wsrc = self.inp[nm][l].rearrange("(kc p) n -> p kc n", p=128)
                for cg in range(ncol // 512):
                    s.dma("pool", self.chan("wl", 4), wt[:, :, cg * 512:(cg + 1) * 512], wsrc[:, :, cg * 512:(cg + 1) * 512], writes=["%s%d" % (key, cg)])
            self.load_ln("ln_x_g", "ln_x_b", l)
            xTb = [SB("xTb", [128, 8, BLK], BF16) for _ in range(2)]
            memin = SB("memin", [128, D], F32)
            memT = SB("memT", [128, 8, MEM], BF16)
            KTs = [SB("KT", [128, 8, MEM], BF16) for _ in range(2)]
            Vs = [SB("V", [128, 2, D], BF16) for _ in range(2)]
            qT = [SB("qT", [128, 8, BLK], BF16) for _ in range(2)]
            Ps = [SB("P", [128, 4, MEM], F32) for _ in range(2)]
            Pn = [SB("Pn", [128, 4, MEM], BF16) for _ in range(2)]
            PT = [SB("PT", [128, 8, 128], BF16) for _ in range(2)]
            oT = [SB("oT", [128, 8, 128], BF16) for _ in range(2)]
            sm = [SB("sm", [128, 12], F32) for _ in range(2)]
            ep = self.ep_alloc(st, router=True, nt=3)

            def X0(t):
                if t % TB:
                    return
                b, ti0 = divmod(t, TPB)
                r = (t // TB) % 2
                if ti0 == 0:
                    for mt in range(2):
                        s.dma("sp", self.chan("xl", 3), memin[:], self.inp["mem"][b * MEM + mt * 128: b * MEM + (mt + 1) * 128, :], writes=["memin"])
                        for hf in range(2):
                            bk = self.bank(); pk = "ps%d" % bk; pz = self.ps[bk]
                            for c in range(4):
                                kc = hf * 4 + c
                                s.op("pe", lambda e, pz=pz, c=c, kc=kc: e.transpose(pz[:, c * 128:(c + 1) * 128], memin[:, kc * 128:(kc + 1) * 128], self.ident_f[:]),
                                     reads=["memin", "ident_f"], writes=[pk])
                            s.op("act", lambda e, pz=pz, hf=hf, mt=mt: e.copy(memT[:, hf * 4:(hf + 1) * 4, mt * 128:(mt + 1) * 128], pz[:].rearrange("p (c t) -> p c t", c=4)),
                                 reads=[pk], writes=["memT"])
                    for c in range(8):
                        bk = self.bank(); pk = "ps%d" % bk; pz = self.ps[bk]
                        self.mm_acc(pz[:, 0:MEM], pk, lambda kc, c=c: wkv[:, kc, c * 128:(c + 1) * 128], lambda kc: memT[:, kc, :], 8, ["memT", "wkv%d" % (c // 4)])
                        s.op("act", lambda e, pz=pz, c=c, b=b: e.mul(KTs[b][:, c, :], pz[:, 0:MEM], XSCALE), reads=[pk], writes=["KT%d" % b])
                    for mt in range(2):
                        for hf in range(2):
                            bk = self.bank(); pk = "ps%d" % bk; pz = self.ps[bk]
                            self.mm_acc(pz[:], pk, lambda kc, mt=mt: memT[:, kc, mt * 128:(mt + 1) * 128], lambda kc, hf=hf: wkv[:, kc, 1024 + hf * 512:1024 + (hf + 1) * 512], 8,
                                        ["memT", "wkv%d" % (2 + hf)])
                            s.op("dve", lambda e, pz=pz, mt=mt, hf=hf, b=b: e.tensor_copy(out=Vs[b][:, mt, hf * 512:(hf + 1) * 512], in_=pz[:]), reads=[pk], writes=["V%d" % b])
                xk = ["xTb%d_%d" % (r, i) for i in range(TB)]
                for c in range(8):
                    bk = self.bank(); pk = "ps%d" % bk; pz = self.ps[bk]
                    self.mm_acc(pz[:, 0:BLK], pk, lambda kc, c=c: wq[:, kc, c * 128:(c + 1) * 128], lambda kc, r=r: xTb[r][:, kc, :], 8, xk + ["wq%d" % (c // 4)])
                    if c % 2:
                        s.op("act", lambda e, pz=pz, c=c, r=r: e.copy(qT[r][:, c, :], pz[:, 0:BLK]), reads=[pk], writes=["qT%d_%d" % (r, c)])
                    else:
                        s.op("dve", lambda e, pz=pz, c=c, r=r: e.tensor_copy(out=qT[r][:, c, :], in_=pz[:, 0:BLK]), reads=[pk], writes=["qT%d_%d" % (r, c)])

            def X0x(t):
                if t % TB:
                    return
                r = (t // TB) % 2
                for i in range(TB):
                    self.load_xT_tile(xTb[r][:, :, i * 128:(i + 1) * 128], t + i, ["xTb%d_%d" % (r, i)])

            def X1a(t):
                r = (t // TB) % 2
                i = t % TB
                p2 = t % 2
                bq = t // TPB
                KT = KTs[bq]
                P = Ps[p2]
                cols = slice(i * 128, (i + 1) * 128)
                sm_ = sm[p2]
                pS, pkS = [], []
                for bb in range(2):
                    bk = self.bank(); pkS.append("ps%d" % bk); pS.append(self.ps[bk][:].rearrange("p (h m) -> p h m", h=2))
                for h in range(4):
                    for hh in range(2):
                        s.op("pe", lambda e, h=h, hh=hh, o_ap=pS[h // 2][:, h % 2, :]: e.matmul(o_ap, qT[r][:, 2 * h + hh, cols], KT[:, 2 * h + hh, :], start=(hh == 0), stop=(hh == 1)),
                             reads=["qT%d_%d" % (r, 2 * h + hh), "KT%d" % bq], writes=[pkS[h // 2]])
                for bb in range(2):
                    s.op("dve", lambda e, bb=bb, ps_=pS[bb]: e.tensor_reduce(out=sm_[:, 2 * bb:2 * bb + 2], in_=ps_, axis=AX.X, op=ALU.max, negate=True), reads=[pkS[bb]], writes=["sm_mx%d" % p2])
                for h in range(4):
                    s.op("act", lambda e, h=h, ps_=pS[h // 2]: e.activation(out=P[:, h, :], in_=ps_[:, h % 2, :], func=AF.Exp, bias=sm_[:, h:h + 1], scale=1.0, accum_out=sm_[:, 4 + h:5 + h]),
                         reads=[pkS[h // 2], "sm_mx%d" % p2], writes=["P%d_%d" % (p2, h), "sm_s%d_%d" % (p2, h)])

            def X1b(t):
                p2 = t % 2
                P = Ps[p2]
                sm_ = sm[p2]
                s.op("dve", lambda e: e.reciprocal(out=sm_[:, 8:12], in_=sm_[:, 4:8]), reads=["sm_s%d_%d" % (p2, h) for h in range(4)], writes=["sm_r%d" % p2])
                for h in range(4):
                    if h % 2:
                        s.op("act", lambda e, h=h: e.activation(out=Pn[p2][:, h, :], in_=P[:, h, :], func=AF.Copy, scale=sm_[:, 8 + h:9 + h]), reads=["P%d_%d" % (p2, h), "sm_r%d" % p2], writes=["Pn%d_%d" % (p2, h)])
                    else:
                        s.op("dve", lambda e, h=h: e.tensor_scalar(out=Pn[p2][:, h, :], in0=P[:, h, :], scalar1=sm_[:, 8 + h:9 + h], scalar2=None, op0=ALU.mult),
                             reads=["P%d_%d" % (p2, h), "sm_r%d" % p2], writes=["Pn%d_%d" % (p2, h)])

            def X1c(t):
                p2 = t % 2
                bk = self.bank(); pk = "ps%d" % bk; pzb = self.psb[bk]
                for h in range(4):
                    for mc in range(2):
                        s.op("pe", lambda e, pzb=pzb, h=h, mc=mc: e.transpose(pzb[:, (2 * h + mc) * 128:(2 * h + mc + 1) * 128], Pn[p2][:, h, mc * 128:(mc + 1) * 128], self.ident_b[:]),
                             reads=["Pn%d_%d" % (p2, h), "ident_b"], writes=[pk])
                s.op("act", lambda e, pzb=pzb: e.copy(PT[p2][:], pzb[:].rearrange("p (c t) -> p c t", c=8)), reads=[pk], writes=["PT%d" % p2])

            def X2(t):
                p2 = t % 2
                bq = t // TPB
                V = Vs[bq]
                for hf in range(2):
                    bk = self.bank(); pk = "ps%d" % bk; pz = self.ps[bk]
                    for c in range(4):
                        cc = hf * 4 + c
                        h, hh = divmod(cc, 2)
                        for mc in range(2):
                            s.op("pe", lambda e, pz=pz, c=c, h=h, hh=hh, mc=mc: e.matmul(pz[:, c * 128:(c + 1) * 128], V[:, mc, h * 256 + hh * 128:h * 256 + (hh + 1) * 128], PT[p2][:, 2 * h + mc, :],
                                                                                    start=(mc == 0), stop=(mc == 1)), reads=["V%d" % bq, "PT%d" % p2], writes=[pk])
                    if hf:
                        s.op("dve", lambda e, pz=pz, hf=hf: e.tensor_copy(out=oT[p2][:, hf * 4:(hf + 1) * 4, :], in_=pz[:].rearrange("p (c t) -> p c t", c=4)), reads=[pk], writes=["oT%d_%d" % (p2, hf)])
                    else:
                        s.op("act", lambda e, pz=pz, hf=hf: e.copy(oT[p2][:, hf * 4:(hf + 1) * 4, :], pz[:].rearrange("p (c t) -> p c t", c=4)), reads=[pk], writes=["oT%d_%d" % (p2, hf)])

            def X3(t):
                p2 = t % 2
                halves, hkeys = [], []
                for hf in range(2):
                    bk = self.bank(); pk = "ps%d" % bk; pz = self.ps[bk]
                    self.mm_acc(pz[:], pk, lambda kc: oT[p2][:, kc, :], lambda kc, hf=hf: wo[:, kc, hf * 512:(hf + 1) * 512], 8, ["oT%d_0" % p2, "oT%d_1" % p2, "wo%d" % hf])
                    halves.append(pz[:]); hkeys.append(pk)
                self.ep1(ep, t, halves, hkeys, src, sname)

            def X3b(t):
                self.ep1b(ep, t)

            def X4a(t):
                self.ep2a(ep, t, dst, dname)

            def X4b(t):
                self.ep2b(ep, t, router=True)

            self.run_pipe(NT, [(-4, X0x, "blk"), (-2, X0, "blk"), (0, X1a), (1, X1b), (2, X1c), (3, X2), (4, X3), (5, X3b), (6, X4a), (7, X4b)])
            self.barrier()

    def moe(self, l, src, sname, dst, dname):
        import contextlib
        s, nc = self.s, self.nc
        with contextlib.ExitStack() as st:
            SB = lambda n, shp, dt: self.sb(st, n, shp, dt)
            yacc = SB("yacc", [128, TPB, D], F32)
            xTm = SB("xTm", [128, 8, S], BF16)
            wg = [SB("wg", [128, 8, DE], BF16) for _ in range(2)]
            wu = [SB("wu", [128, 8, DE], BF16) for _ in range(2)]
            wd = [SB("wd", [128, 2, D], BF16) for _ in range(2)]
            sg = [SB("sg", [128, 512], F32) for _ in range(2)]
            hT = [SB("hT", [128, 2, 512], BF16) for _ in range(2)]
            ep = self.ep_alloc(st, nt=3)
            self.load_ln("ln_moe_g", "ln_moe_b", l)
            n = 0
            pending_ep = []
            for b in range(NB):
                for ti in range(TPB):
                    self.load_xT_tile(xTm[:, :, ti * 128:(ti + 1) * 128], b * TPB + ti, ["xTm_%d" % ti])
                for ex in range(E):
                    r = n % 2
                    n += 1
                    s.dma("pool", "wg%d" % r, wg[r][:], self.inp["w_gate"][l, ex].rearrange("(kc p) n -> p kc n", p=128), writes=["wg%d" % r])
                    s.dma("pool", "wu%d" % r, wu[r][:], self.inp["w_up"][l, ex].rearrange("(kc p) n -> p kc n", p=128), writes=["wu%d" % r])
                    s.dma("pool", "wd%d" % r, wd[r][:], self.inp["w_down"][l, ex].rearrange("(kc p) n -> p kc n", p=128), writes=["wd%d" % r])
                    hn = 0
                    for j in range(4):
                        if ex == 0 and pending_ep:
                            fn_ = pending_ep.pop(0)
                            fn_()
                        tok0 = j * 512
                        xk = ["xTm_%d" % ti for ti in range(4 * j, 4 * j + 4)]
                        hr = hn % 2
                        hn += 1
                        for c in range(2):
                            bkg = self.bank(); pkg = "ps%d" % bkg; pg = self.ps[bkg]
                            self.mm_acc(pg[:], pkg, lambda kc, c=c, r=r: wg[r][:, kc, c * 128:(c + 1) * 128], lambda kc, tok0=tok0: xTm[:, kc, tok0:tok0 + 512], 8, xk + ["wg%d" % r])
                            bku = self.bank(); pku = "ps%d" % bku; pu = self.ps[bku]
                            self.mm_acc(pu[:], pku, lambda kc, c=c, r=r: wu[r][:, kc, c * 128:(c + 1) * 128], lambda kc, tok0=tok0: xTm[:, kc, tok0:tok0 + 512], 8, xk + ["wu%d" % r])
                            s.op("act", lambda e, pg=pg, c=c: e.activation(out=sg[c][:], in_=pg[:], func=AF.Silu), reads=[pkg], writes=["sg%d" % c])
                            s.op("dve", lambda e, pu=pu, c=c, hr=hr: e.tensor_tensor(out=hT[hr][:, c, :], in0=sg[c][:], in1=pu[:], op=ALU.mult), reads=["sg%d" % c, pku], writes=["hT%d_%d" % (hr, c)])
                        for i in range(4):
                            ti = 4 * j + i
                            t = b * TPB + ti
                            cols = slice(i * 128, (i + 1) * 128)
                            for hf in range(2):
                                bk = self.bank(); pk = "ps%d" % bk; pz = self.ps[bk]
                                for c in range(2):
                                    s.op("pe", lambda e, pz=pz, c=c, hr=hr, r=r, hf=hf, cols=cols: e.matmul(pz[:], hT[hr][:, c, cols], wd[r][:, c, hf * 512:(hf + 1) * 512], start=(c == 0), stop=(c == 1)),
                                         reads=["hT%d_%d" % (hr, c), "wd%d" % r], writes=[pk])
                                ya = yacc[:, ti, hf * 512:(hf + 1) * 512]
                                yk = "y%d_%d" % (ti, hf)
                                wcol = self.wgt[:, t, ex:ex + 1]
                                if ex == 0:
                                    s.op("dve", lambda e, pz=pz, ya=ya, wcol=wcol: e.tensor_scalar(out=ya, in0=pz[:], scalar1=wcol, scalar2=None, op0=ALU.mult), reads=[pk, "wgt%d" % t], writes=[yk])
                                else:
                                    s.op("dve", lambda e, pz=pz, ya=ya, wcol=wcol: e.scalar_tensor_tensor(out=ya, in0=pz[:], scalar=wcol, in1=ya, op0=ALU.mult, op1=ALU.add),
                                         reads=[pk, "wgt%d" % t, yk], writes=[yk])

                def M1(ti, b=b):
                    t = b * TPB + ti
                    self.ep1(ep, t, [yacc[:, ti, 0:512], yacc[:, ti, 512:1024]], ["y%d_0" % ti, "y%d_1" % ti], src, sname)

                def M2(ti, b=b):
                    self.ep2a(ep, b * TPB + ti, dst, dname)

                def M3(ti, b=b):
                    self.ep2b(ep, b * TPB + ti)
                if b == NB - 1:
                    self.run_pipe(TPB, [(0, M1), (1, M2), (2, M3)])
                else:
                    for q in range(4):
                        pending_ep.append(lambda q=q, M1=M1, M2=M2, M3=M3: self.run_pipe_range(4 * q, 4 * q + 4, [(0, M1), (1, M2), (2, M3)]))
            self.barrier()


I32 = mybir.dt.int32
NSLOT = NT + 4
XSW = D + 4


class Prog3(Prog2):
    POOLS = None

    def __init__(self, **kw):
        super().__init__(**kw)
        for name in ("w_gate_r", "w_up_r", "w_down_r"):
            for l in range(2):
                self.inp["%s%d" % (name, l)] = self.nc.dram_tensor("%s%d" % (name, l), [512, 8192], F32, kind="ExternalInput").ap()

    def setup(self):
        super().setup()
        nc, s = self.nc, self.s
        A = nc.alloc_sbuf_tensor
        self.ohs = A("ohs", [128, NT, 4], F32)
        self.wg4s = A("wg4s", [128, NT, 4], F32)
        self.striu = A("striu", [128, 128], F32)
        self.thr = A("thr", [128, 32], F32)
        self.jidx = A("jidx", [128, NSLOT], F32)
        self.ii32 = A("ii32", [128, NSLOT], I32)
        self.desti = A("desti", [128, NT], I32)
        self.tgi = A("tgi", [128, NSLOT], I32)
        self.chgi = A("chgi", [128, NSLOT], I32)
        self.idxw = A("idxw", [128, NSLOT], I32)
        self.pidx = A("pidx", [128, NSLOT], F32)
        self.pi32 = A("pi32", [128, NSLOT], I32)
        self.xs = nc.dram_tensor("xs_sorted", [NSLOT * 128, XSW], F32, kind="Internal").ap()
        self.ys = nc.dram_tensor("ys_sorted", [NSLOT * 128, D], F32, kind="Internal").ap()
        s.op("pool", lambda e: e.affine_select(out=self.striu[:], in_=self.ones_f[:], pattern=[[1, 128]],
                                               compare_op=ALU.is_gt, fill=0.0, base=0, channel_multiplier=-1),
             reads=["ones_f"], writes=["striu"])
        s.op("pool", lambda e: e.iota(self.ii32[:], pattern=[[1, NSLOT]], base=0, channel_multiplier=0), writes=["ii32"])
        s.op("dve", lambda e: e.tensor_copy(out=self.jidx[:], in_=self.ii32[:]), reads=["ii32"], writes=["jidx"])
        s.op("pool", lambda e: e.iota(self.pi32[:], pattern=[[0, NSLOT]], base=0, channel_multiplier=1), writes=["pi32"])
        s.op("dve", lambda e: e.tensor_copy(out=self.pidx[:], in_=self.pi32[:]), reads=["pi32"], writes=["pidx"])
        s.op("dve", lambda e: e.tensor_scalar(out=self.thr[:], in0=self.jidx[:, 0:32], scalar1=128.0, scalar2=None, op0=ALU.mult), reads=["jidx"], writes=["thr"])

    def ep2b(self, ep, t, router=False):
        if not router:
            return super().ep2b(ep, t)
        s = self.s
        i = t % ep["nt"]
        tt = ep["t"][i]
        tk = ["ep_t0%d" % i, "ep_t1%d" % i]
        xTf = ep["xTf"][0]
        for hf in range(2):
            bk = self.bank(); pk = "ps%d" % bk; pt = self.ps[bk]
            for c in range(4):
                kc = hf * 4 + c
                s.op("pe", lambda e, pt=pt, c=c, kc=kc: e.transpose(pt[:, c * 128:(c + 1) * 128], tt[:, kc * 128:(kc + 1) * 128], self.ident_f[:]), reads=tk + ["ident_f"], writes=[pk])
            srcv = pt[:].rearrange("p (c t) -> p c t", c=4)
            if hf:
                s.op("act", lambda e, srcv=srcv, hf=hf: e.copy(xTf[:, hf * 4:(hf + 1) * 4, :], srcv), reads=[pk], writes=["ep_xTf%d" % hf])
            else:
                s.op("dve", lambda e, srcv=srcv, hf=hf: e.tensor_copy(out=xTf[:, hf * 4:(hf + 1) * 4, :], in_=srcv), reads=[pk], writes=["ep_xTf%d" % hf])
        self.route(ep, t % 2, t, xTf, ["ep_xTf0", "ep_xTf1"])

    def bc_reg(self, e):
        if getattr(self, "_bc_reg", None) is None:
            self._bc_reg = e.to_reg(511)
        return self._bc_reg

    def route(self, ep, i, t, xTf, xkeys):
        super().route(ep, i, t, xTf, xkeys)
        s = self.s
        sc, sel, eq, m, t4, ge, msk, gsel = ep["r"][i]
        R = lambda n: "rt_%s%d" % (n, i)
        s.op("dve", lambda e: e.tensor_copy(out=self.ohs[:, t, :], in_=t4[:, 0:4]), reads=[R("t4")], writes=["ohs%d" % t])
        s.op("dve", lambda e: e.tensor_reduce(out=self.wg4s[:, t, :], in_=self.wgt[:, t, :].rearrange("p (g k) -> p k g", g=4), axis=AX.X, op=ALU.add),
             reads=["wgt%d" % t], writes=["wg4s%d" % t])

    def moe(self, l, src, sname, dst, dname):
        import contextlib
        s, nc = self.s, self.nc
        with contextlib.ExitStack() as st:
            SB = lambda n, shp, dt: self.sb(st, n, shp, dt)
            wgas = [SB("wga", [128, 32, DE], BF16) for _ in range(2)]
            wuas = [SB("wua", [128, 32, DE], BF16) for _ in range(2)]
            wdas = [SB("wda", [128, 8, D], BF16) for _ in range(2)]
            xsb = [SB("xsb", [128, XSW], F32) for _ in range(5)]
            xsj = [SB("xsj", [128, XSW], F32) for _ in range(3)]
            xgT = [SB("xgT", [128, 8, 128], BF16) for _ in range(2)]
            sgu = [SB("sgu", [128, 256], F32) for _ in range(2)]
            hTs = [[SB("hTs", [128, 256], BF16) for _ in range(4)] for _ in range(2)]
            yt = [SB("yt", [128, D], F32) for _ in range(2)]
            yg = [SB("yg", [128, D], F32) for _ in range(3)]
            csg = SB("csg", [128, 4, 32], F32)
            inc = SB("inc", [128, 4, 32], F32)
            ones32 = SB("ones32", [128, 32], F32)
            cmp_ = SB("cmp", [128, 4, 32], F32)
            sm = SB("msm", [128, 32], F32)
            tmpT = SB("tmpT", [128, NT, 4], F32)
            dsum = SB("dsum", [128, NT, 4], F32)
            destf = SB("destf", [128, NT], F32)
            tgf = SB("tgf", [128, NSLOT], F32)
            tgc = SB("tgc", [128, NSLOT], F32)
            chgf = SB("chgf", [128, NSLOT], F32)
            ep = self.ep_alloc(st, nt=3)
            self.load_ln("ln_moe_g", "ln_moe_b", l)

            ohk = ["ohs%d" % t for t in range(NT)]
            ohs_v = self.ohs[:].rearrange("p t g -> p (t g)")
            bkA = self.bank(); pkA = "ps%d" % bkA; psA = self.ps[bkA]
            bkB = self.bank(); pkB = "ps%d" % bkB; psB = self.ps[bkB]
            s.op("pe", lambda e: e.matmul(psA[:, 0:128], self.ones_f[:], ohs_v, start=True, stop=True), reads=ohk + ["ones_f"], writes=[pkA])
            s.op("pe", lambda e: e.matmul(psB[:, 0:128], self.striu[:], ohs_v, start=True, stop=True), reads=ohk + ["striu"], writes=[pkB])
            s.op("dve", lambda e: e.memset(ones32[:], 1.0), writes=["ones32"])
            s.op("dve", lambda e: e.tensor_copy(out=csg[:], in_=psA[:, 0:128].rearrange("p (t g) -> p g t", g=4)), reads=[pkA], writes=["csg"])
            for g in range(4):
                s.op("dve", lambda e, g=g: e.tensor_tensor_scan(out=inc[:, g, :], data0=ones32[:], data1=csg[:, g, :], initial=0.0, op0=ALU.mult, op1=ALU.add),
                     reads=["csg", "ones32"], writes=["inc"])
            s.op("dve", lambda e: e.tensor_copy(out=sm[:, 0:4], in_=inc[:, :, 31]), reads=["inc"], writes=["msm"])
            s.op("dve", lambda e: e.tensor_tensor(out=cmp_[:], in0=self.thr[:].unsqueeze(1).broadcast_to([128, 4, 32]), in1=sm[:, 0:4].unsqueeze(2).broadcast_to([128, 4, 32]), op=ALU.is_lt),
                 reads=["thr", "msm"], writes=["cmp"])
            s.op("dve", lambda e: e.tensor_reduce(out=sm[:, 4:8], in_=cmp_[:], axis=AX.X, op=ALU.add), reads=["cmp", "msm"], writes=["msm"])
            s.op("dve", lambda e: e.memset(sm[:, 8:9], 0.0), reads=["msm"], writes=["msm"])
            for g in range(1, 4):
                s.op("dve", lambda e, g=g: e.tensor_tensor(out=sm[:, 8 + g:9 + g], in0=sm[:, 7 + g:8 + g], in1=sm[:, 3 + g:4 + g], op=ALU.add), reads=["msm"], writes=["msm"])
            s.op("dve", lambda e: e.tensor_tensor(out=sm[:, 12:16], in0=sm[:, 8:12], in1=sm[:, 4:8], op=ALU.add), reads=["msm"], writes=["msm"])
            s.op("dve", lambda e: e.tensor_scalar(out=sm[:, 16:20], in0=sm[:, 8:12], scalar1=128.0, scalar2=None, op0=ALU.mult), reads=["msm"], writes=["msm"])
            s.op("dve", lambda e: e.tensor_tensor(out=inc[:], in0=inc[:], in1=csg[:], op=ALU.subtract), reads=["inc", "csg"], writes=["inc"])
            s.op("dve", lambda e: e.tensor_tensor(out=tmpT[:].rearrange("p t g -> p g t"), in0=inc[:], in1=sm[:, 16:20].unsqueeze(2).broadcast_to([128, 4, 32]), op=ALU.add),
                 reads=["inc", "msm"], writes=["tmpT"])
            s.op("dve", lambda e: e.tensor_tensor(out=dsum[:].rearrange("p t g -> p (t g)"), in0=psB[:, 0:128], in1=tmpT[:].rearrange("p t g -> p (t g)"), op=ALU.add),
                 reads=[pkB, "tmpT"], writes=["dsum"])
            s.op("dve", lambda e: e.tensor_tensor(out=dsum[:], in0=dsum[:], in1=self.ohs[:], op=ALU.mult), reads=["dsum"] + ohk, writes=["dsum"])
            s.op("dve", lambda e: e.tensor_reduce(out=destf[:], in_=dsum[:], axis=AX.X, op=ALU.add), reads=["dsum"], writes=["destf"])
            s.op("dve", lambda e: e.tensor_copy(out=self.desti[:], in_=destf[:]), reads=["destf"], writes=["desti"])
            s.op("dve", lambda e: e.tensor_scalar(out=tgf[:], in0=self.jidx[:], scalar1=sm[:, 12:13], scalar2=None, op0=ALU.is_ge), reads=["jidx", "msm"], writes=["tgf"])
            for g in range(1, 3):
                s.op("dve", lambda e, g=g: e.tensor_scalar(out=tgc[:], in0=self.jidx[:], scalar1=sm[:, 12 + g:13 + g], scalar2=None, op0=ALU.is_ge), reads=["jidx", "msm"], writes=["tgc"])
                s.op("dve", lambda e: e.tensor_tensor(out=tgf[:], in0=tgf[:], in1=tgc[:], op=ALU.add), reads=["tgf", "tgc"], writes=["tgf"])
            s.op("dve", lambda e: e.memset(chgf[:, 0:2], 1.0), writes=["chgf"])
            s.op("dve", lambda e: e.tensor_tensor(out=chgf[:, 2:NSLOT], in0=tgf[:, 2:NSLOT], in1=tgf[:, 0:NSLOT - 2], op=ALU.subtract), reads=["tgf", "chgf"], writes=["chgf"])
            s.op("dve", lambda e: e.tensor_copy(out=self.tgi[:], in_=tgf[:]), reads=["tgf"], writes=["tgi"])
            s.op("dve", lambda e: e.tensor_copy(out=self.chgi[:], in_=chgf[:]), reads=["chgf"], writes=["chgi"])
            s.op("dve", lambda e: e.tensor_scalar(out=chgf[:], in0=chgf[:], scalar1=0.0, scalar2=None, op0=ALU.is_gt), reads=["chgf", "chgi"], writes=["chgf"])
            s.op("dve", lambda e: e.tensor_scalar(out=chgf[:], in0=chgf[:], scalar1=-1.0e6, scalar2=1.0e6, op0=ALU.mult, op1=ALU.add), reads=["chgf"], writes=["chgf"])
            s.op("dve", lambda e: e.scalar_tensor_tensor(out=tgc[:], in0=tgf[:], scalar=128.0, in1=self.pidx[:], op0=ALU.mult, op1=ALU.add), reads=["tgf", "pidx", "tgc"], writes=["tgc"])
            s.op("dve", lambda e: e.tensor_tensor(out=tgc[:], in0=tgc[:], in1=chgf[:], op=ALU.add), reads=["tgc", "chgf"], writes=["tgc"])
            s.op("dve", lambda e: e.tensor_copy(out=self.idxw[:], in_=tgc[:]), reads=["tgc"], writes=["idxw"])

            for t in range(NT):
                xb = xsb[t % 5]
                XK = "xsb%d" % (t % 5)
                s.dma("sp", self.chan("xl5", 5), xb[:, 0:D], src[t * 128:(t + 1) * 128, :], reads=["dram_%s_%d" % (sname, t)], writes=[XK])
                s.op("act", lambda e, xb=xb, t=t: e.copy(xb[:, D:XSW], self.wg4s[:, t, :]), reads=["wg4s%d" % t], writes=[XK])
                op = s.dma("pool", self.chan("scat", 5), self.xs, xb[:], reads=[XK, "desti"], writes=["xs_w%d" % t])
                op.fn = (lambda e, xb=xb, t=t: e.indirect_dma_start(out=self.xs[:, :], out_offset=bass.IndirectOffsetOnAxis(ap=self.desti[:, t:t + 1], axis=0),
                                                                   in_=xb[:, :], in_offset=None))

            wsrc = {"g": self.inp["w_gate"], "u": self.inp["w_up"], "d": self.inp["w_down"]}

            def C0(j):
                xj = xsj[j % 3]
                XJ = "xsj%d" % (j % 3)
                s.dma("sp", self.chan("xl", 3), xj[:], self.xs[j * 128:(j + 1) * 128, :], reads=["xs_w%d" % tt_ for tt_ in range(NT)], writes=[XJ])
                r = j % 2
                for hf in range(2):
                    bk = self.bank(); pk = "ps%d" % bk; pt = self.ps[bk]
                    for c in range(4):
                        kc = hf * 4 + c
                        s.op("pe", lambda e, pt=pt, c=c, kc=kc, xj=xj: e.transpose(pt[:, c * 128:(c + 1) * 128], xj[:, kc * 128:(kc + 1) * 128], self.ident_f[:]),
                             reads=[XJ, "ident_f"], writes=[pk])
                    srcv = pt[:].rearrange("p (c t) -> p c t", c=4)
                    if hf:
                        s.op("act", lambda e, srcv=srcv, r=r, hf=hf: e.copy(xgT[r][:, hf * 4:(hf + 1) * 4, :], srcv), reads=[pk], writes=["xgT%d_%d" % (r, hf)])
                    else:
                        s.op("dve", lambda e, srcv=srcv, r=r, hf=hf: e.tensor_copy(out=xgT[r][:, hf * 4:(hf + 1) * 4, :], in_=srcv), reads=[pk], writes=["xgT%d_%d" % (r, hf)])

            def CW(j, only_down=False):
                r = j % 2
                wga, wua, wda = wgas[r], wuas[r], wdas[r]
                for wn, wt, wk in ((("w_down_r", wda, "wda%d" % r),) if only_down else (("w_gate_r", wga, "wga%d" % r), ("w_up_r", wua, "wua%d" % r))):
                    op = s.dma("pool", "wsp_" + wk, wt[:], self.inp[wn + str(l)], reads=["idxw"], writes=[wk])
                    op.fn = (lambda e, wn=wn, wt=wt, j=j: e.indirect_dma_start(out=wt[:].rearrange("p a n -> p (a n)"), out_offset=None, in_=self.inp[wn + str(l)][:, :],
                                                                              in_offset=bass.IndirectOffsetOnAxis(ap=self.idxw[:, j:j + 1], axis=0),
                                                                              bounds_check=self.bc_reg(e), oob_is_err=False))

            def C1(j):
                r = j % 2
                wga, wua = wgas[r], wuas[r]
                for ex in range(4):
                    bk = self.bank(); pk = "ps%d" % bk; pz = self.ps[bk]
                    for q, (wt, wk) in enumerate(((wga, "wga%d" % r), (wga, "wga%d" % r), (wua, "wua%d" % r), (wua, "wua%d" % r))):
                        c = q % 2
                        self.mm_acc(pz[:, q * 128:(q + 1) * 128], pk, lambda kc, wt=wt, ex=ex, c=c: wt[:, ex * 8 + kc, c * 128:(c + 1) * 128], lambda kc, r=r: xgT[r][:, kc, :], 8,
                                    ["xgT%d_0" % r, "xgT%d_1" % r, wk])
                    sg_ = sgu[ex % 2]
                    s.op("act", lambda e, pz=pz, sg_=sg_: e.activation(out=sg_[:], in_=pz[:, 0:256], func=AF.Silu), reads=[pk], writes=["sgu%d" % (ex % 2)])
                    s.op("dve", lambda e, pz=pz, sg_=sg_, r=r, ex=ex: e.tensor_tensor(out=hTs[r][ex][:], in0=sg_[:], in1=pz[:, 256:512], op=ALU.mult), reads=["sgu%d" % (ex % 2), pk], writes=["hTs%d_%d" % (r, ex)])

            def C2(j):
                r = j % 2
                wda = wdas[r]
                xj = xsj[j % 3]
                XJ = "xsj%d" % (j % 3)
                y_ = yt[r]
                for ex in range(4):
                    for hf in range(2):
                        bk = self.bank(); pk = "ps%d" % bk; pz = self.ps[bk]
                        for c in range(2):
                            s.op("pe", lambda e, pz=pz, c=c, ex=ex, hf=hf, r=r: e.matmul(pz[:], hTs[r][ex][:, c * 128:(c + 1) * 128], wda[:, ex * 2 + c, hf * 512:(hf + 1) * 512], start=(c == 0), stop=(c == 1)),
                                 reads=["hTs%d_%d" % (r, ex), "wda%d" % r], writes=[pk])
                        ya = y_[:, hf * 512:(hf + 1) * 512]
                        yk = "yt%d_%d" % (r, hf)
                        wcol = xj[:, D + ex:D + ex + 1]
                        if ex == 0:
                            s.op("dve", lambda e, pz=pz, ya=ya, wcol=wcol: e.tensor_scalar(out=ya, in0=pz[:], scalar1=wcol, scalar2=None, op0=ALU.mult), reads=[pk, XJ], writes=[yk])
                        else:
                            s.op("dve", lambda e, pz=pz, ya=ya, wcol=wcol: e.scalar_tensor_tensor(out=ya, in0=pz[:], scalar=wcol, in1=ya, op0=ALU.mult, op1=ALU.add), reads=[pk, XJ, yk], writes=[yk])
                s.dma("pool", self.chan("yst", 2), self.ys[j * 128:(j + 1) * 128, :], y_[:], reads=["yt%d_0" % r, "yt%d_1" % r], writes=["ys_%d" % j])

            self.run_pipe(NSLOT, [(-2, CW), (-1, C0), (-1, lambda j: CW(j, only_down=True)), (0, C1), (1, C2)])

            ysk = ["ys_%d" % j for j in range(NSLOT)]

            def M0(t):
                yg_ = yg[t % 3]
                YK = "yg%d" % (t % 3)
                op = s.dma("pool", self.chan("gath", 3), yg_[:], self.ys, reads=ysk + ["desti"], writes=[YK])
                op.fn = (lambda e, yg_=yg_, t=t: e.indirect_dma_start(out=yg_[:, :], out_offset=None, in_=self.ys[:, :],
                                                                     in_offset=bass.IndirectOffsetOnAxis(ap=self.desti[:, t:t + 1], axis=0)))

            def M1(t):
                yg_ = yg[t % 3]
                YK = "yg%d" % (t % 3)
                self.ep1(ep, t, [yg_[:, 0:512], yg_[:, 512:1024]], [YK, YK], src, sname)

            def M1b(t):
                self.ep1b(ep, t)

            def M2(t):
                self.ep2a(ep, t, dst, dname)

            def M3(t):
                if not self.is_last_stage:
                    self.ep2b(ep, t)
            self.run_pipe(NT, [(-2, M0), (0, M1), (1, M1b), (2, M2), (3, M3)])
            self.barrier()


_CACHE = {}


def _program(**kw):
    key = tuple(sorted(kw.items()))
    if key not in _CACHE:
        p = Prog3(**kw)
        p.counts = p.build()
        _CACHE[key] = p
    return _CACHE[key]


def make_in_maps(inputs, n_cores=8):
    maps = []
    x = np.ascontiguousarray(inputs["x"], dtype=np.float32)
    mem = np.ascontiguousarray(inputs["mem"], dtype=np.float32)
    shared = {}
    for name, shp in PARAM_SPECS:
        shared[name] = np.ascontiguousarray(np.asarray(inputs[name], dtype=np.float32).reshape(shp))
    wg = shared["w_gate"].reshape(2, 4, 4, 8, 128, 256)
    wu = shared["w_up"].reshape(2, 4, 4, 8, 128, 256)
    wd = shared["w_down"].reshape(2, 4, 4, 2, 128, 1024)
    for nm, arr in (("w_gate_r", wg), ("w_up_r", wu), ("w_down_r", wd)):
        r_ = np.ascontiguousarray(arr.transpose(0, 1, 4, 2, 3, 5)).reshape(2, 512, 8192)
        for l in range(2):
            shared["%s%d" % (nm, l)] = r_[l]
    for c in range(n_cores):
        m = dict(shared)
        m["x"] = x[c * NB:(c + 1) * NB].reshape(TOK, D)
        m["mem"] = mem[c * NB:(c + 1) * NB].reshape(NB * MEM, D)
        maps.append(m)
    return maps


def kernel(**inputs):
    p = _program()
    maps = make_in_maps(inputs)
    res = run_bass_kernel_spmd(p.nc, maps, core_ids=list(range(8)))
    outs = [np.asarray(r["out"]).reshape(NB, S, D) for r in res.results]
    return np.concatenate(outs, axis=0).astype(np.float32)
```
